# Optimizing a Trainium2 kernel written in Bass

```python
import jax
import jax.numpy as jnp
from jax import lax
import numpy as np

D_MODEL = 2048
BATCH = 8
SEQ = 2048
DEPTH = 1

MIX_WIDTH = D_MODEL
RWKV_WIDTH = MIX_WIDTH // 2
RWKV_HEAD = 64
RWKV_HEADS = RWKV_WIDTH // RWKV_HEAD
DECAY_LORA = 64
AAA_LORA = 64
GATE_LORA = 160
RWKV_LN_EPS = 64e-5
ATT_WIDTH = MIX_WIDTH - RWKV_WIDTH
ATT_HEAD = 128
ATT_HEADS = ATT_WIDTH // ATT_HEAD
ATT_KV_HEADS = 2
IDX_HEADS = 16
IDX_HEAD = 64
TOPK_MAX = 256
Q_BLOCK = 128
ROPE_THETA = 500000.0
N_MEM = 256
CROSS_HEADS = 4
CROSS_HEAD = 128
D_FF = 4 * D_MODEL
NORM_EPS = 1e-5

RWKV_SPLITS = (RWKV_WIDTH, RWKV_WIDTH, RWKV_WIDTH, DECAY_LORA, AAA_LORA, GATE_LORA)
ATT_SPLITS = (ATT_WIDTH, ATT_KV_HEADS * ATT_HEAD, ATT_KV_HEADS * ATT_HEAD,
              IDX_HEADS * IDX_HEAD, IDX_HEAD, IDX_HEADS)
RWKV_COLS = sum(RWKV_SPLITS)
ATT_COLS = sum(ATT_SPLITS)
IN_COLS = RWKV_COLS + ATT_COLS

kernel_name = 'hybrid_rwkv7_dsa_block'


def split_cols(t, sizes):
    out, start = [], 0
    for s in sizes:
        out.append(t[..., start:start + s])
        start += s
    return out


def rms_norm(x, g):
    xf = x.astype(jnp.float32)
    y = xf * lax.rsqrt(jnp.mean(xf * xf, axis=-1, keepdims=True) + NORM_EPS)
    return (y * g.astype(jnp.float32)).astype(x.dtype)


def partial_rope(x, pos):
    dh = x.shape[-1]
    rot = dh // 4
    half = rot // 2
    inv_freq = ROPE_THETA ** (-jnp.arange(half, dtype=jnp.float32) / half)
    ang = pos.astype(jnp.float32)[..., None] * inv_freq
    cos = jnp.cos(ang)[:, :, None, :]
    sin = jnp.sin(ang)[:, :, None, :]
    xr = x[..., :rot].astype(jnp.float32)
    x1, x2 = xr[..., :half], xr[..., half:]
    rotated = jnp.concatenate([x1 * cos - x2 * sin, x2 * cos + x1 * sin], axis=-1).astype(x.dtype)
    return jnp.concatenate([rotated, x[..., rot:]], axis=-1)


def rwkv7_group(p, mu, w0, w_decay_up, a0, a_up, g_up, k_k, k_a, r_k, lnx_w, lnx_b):
    B, S, _ = p.shape
    H, N = RWKV_HEADS, RWKV_HEAD
    f32 = jnp.float32
    prev = jnp.pad(p, ((0, 0), (1, 0), (0, 0)))[:, :-1]
    p = p + (prev - p) * mu
    r, k, v, xw, xa, xg = split_cols(p, RWKV_SPLITS)
    w = -jax.nn.softplus(-(w0 + jnp.tanh(xw) @ w_decay_up)) - 0.5
    decay = jnp.exp(-jnp.exp(w.astype(f32)))
    a = jax.nn.sigmoid(a0 + xa @ a_up)
    g = jax.nn.sigmoid(xg) @ g_up
    heads = lambda t: t.astype(f32).reshape(B, S, H, N)
    kk = heads(k * k_k)
    kk = kk / jnp.maximum(jnp.sqrt(jnp.sum(kk * kk, axis=-1, keepdims=True)), 1e-12)
    k = k * (1.0 + (a - 1.0) * k_a)
    r4, k4, v4, a4, w4 = heads(r), heads(k), heads(v), heads(a), decay.reshape(B, S, H, N)

    def step(state, inp):
        r_t, w_t, k_t, v_t, kk_t, a_t = inp
        sa = jnp.einsum('bhvk,bhk->bhv', state, -kk_t)
        state = (state * w_t[:, :, None, :]
                 + sa[..., None] * (kk_t * a_t)[:, :, None, :]
                 + v_t[..., None] * k_t[:, :, None, :])
        return state, jnp.einsum('bhvk,bhk->bhv', state, r_t)

    tm = lambda t: jnp.swapaxes(t, 0, 1)
    s0 = jnp.zeros((B, H, N, N), f32)
    _, y = lax.scan(step, s0, (tm(r4), tm(w4), tm(k4), tm(v4), tm(kk), tm(a4)))
    y = tm(y)
    mean = jnp.mean(y, axis=-1, keepdims=True)
    var = jnp.mean(jnp.square(y - mean), axis=-1, keepdims=True)
    yn = ((y - mean) * lax.rsqrt(var + RWKV_LN_EPS)).reshape(B, S, RWKV_WIDTH)
    yn = yn * lnx_w.astype(f32) + lnx_b.astype(f32)
    bonus = (jnp.sum(r4 * k4 * r_k.astype(f32), axis=-1, keepdims=True) * v4).reshape(B, S, RWKV_WIDTH)
    return ((yn + bonus) * g.astype(f32)).astype(p.dtype)


def dsa_group(p, pos):
    B, S, _ = p.shape
    f32 = jnp.float32
    G = ATT_HEADS // ATT_KV_HEADS
    q, k, v, qi, ki, wi = split_cols(p, ATT_SPLITS)
    q = partial_rope(q.reshape(B, S, ATT_HEADS, ATT_HEAD), pos)
    k = partial_rope(k.reshape(B, S, ATT_KV_HEADS, ATT_HEAD), pos)
    v = v.reshape(B, S, ATT_KV_HEADS, ATT_HEAD)
    qi = partial_rope(qi.reshape(B, S, IDX_HEADS, IDX_HEAD), pos)
    ki = partial_rope(ki.reshape(B, S, 1, IDX_HEAD), pos)[:, :, 0].astype(f32)
    wi = wi.astype(f32) * (IDX_HEADS ** -0.5 * IDX_HEAD ** -0.5)
    n_sel = min(TOPK_MAX, S // 4)
    nblk = S // Q_BLOCK
    key_pos = jnp.arange(S)
    gather = jax.vmap(lambda t, i: t[i])

    def to_blocks(t):
        return jnp.swapaxes(t.reshape((B, nblk, Q_BLOCK) + t.shape[2:]), 0, 1)

    def block(args):
        bi, qb, qib, wib = args
        q_pos = bi * Q_BLOCK + jnp.arange(Q_BLOCK)
        rel = jax.nn.relu(jnp.einsum('bqhd,bsd->bqhs', qib.astype(f32), ki))
        score = jnp.einsum('bqhs,bqh->bqs', rel, wib)
        causal = key_pos[None, :] <= q_pos[:, None]
        score = jnp.where(causal[None], score, -1e30)
        _, idx = lax.top_k(score, n_sel)
        valid = idx <= q_pos[None, :, None]
        kg = gather(k, idx)
        vg = gather(v, idx)
        qg = qb.reshape(B, Q_BLOCK, ATT_KV_HEADS, G, ATT_HEAD)
        s = jnp.einsum('bqhgd,bqkhd->bqhgk', qg, kg).astype(f32) * (ATT_HEAD ** -0.5)
        s = jnp.where(valid[:, :, None, None, :], s, -jnp.inf)
        prob = jax.nn.softmax(s, axis=-1).astype(vg.dtype)
        o = jnp.einsum('bqhgk,bqkhd->bqhgd', prob, vg)
        return o.reshape(B, Q_BLOCK, ATT_WIDTH)

    out = lax.map(block, (jnp.arange(nblk), to_blocks(q), to_blocks(qi), to_blocks(wi)))
    return jnp.swapaxes(out, 0, 1).reshape(B, S, ATT_WIDTH)


def memory_cross_attention(hn, mem_n, w_q, w_kv, w_o):
    B, S, _ = hn.shape
    M = mem_n.shape[1]
    q = (hn @ w_q).reshape(B, S, CROSS_HEADS, CROSS_HEAD)
    k, v = split_cols(mem_n @ w_kv, (CROSS_HEADS * CROSS_HEAD, CROSS_HEADS * CROSS_HEAD))
    k = k.reshape(B, M, CROSS_HEADS, CROSS_HEAD)
    v = v.reshape(B, M, CROSS_HEADS, CROSS_HEAD)
    s = jnp.einsum('bshd,bmhd->bhsm', q, k).astype(jnp.float32) * (CROSS_HEAD ** -0.5)
    prob = jax.nn.softmax(s, axis=-1).astype(v.dtype)
    o = jnp.einsum('bhsm,bmhd->bshd', prob, v).reshape(B, S, CROSS_HEADS * CROSS_HEAD)
    return o @ w_o


def setup_inputs(seed: int = 0) -> dict:
    key = jax.random.key(seed)
    ks = jax.random.split(key, 32)
    f32 = jnp.float32
    L = DEPTH

    def dense(k, fan_in, shape):
        return jax.random.normal(k, shape, f32) * fan_in ** -0.5

    def gain(k, shape):
        return 1.0 + 0.02 * jax.random.normal(k, shape, f32)

    return {
        'x': jax.random.normal(ks[0], (BATCH, SEQ, D_MODEL), f32),
        'mem': jax.random.normal(ks[1], (BATCH, N_MEM, D_MODEL), f32),
        'positions': jnp.tile(jnp.arange(SEQ, dtype=jnp.int32)[None, :], (BATCH, 1)),
        'norm_mix': gain(ks[2], (L, D_MODEL)),
        'w_in': dense(ks[3], D_MODEL, (L, D_MODEL, IN_COLS)),
        'rwkv_mu': jax.random.uniform(ks[4], (L, RWKV_COLS), f32),
        'w_decay0': jax.random.uniform(ks[5], (L, RWKV_WIDTH), f32, -6.0, 1.0),
        'w_decay_up': dense(ks[6], DECAY_LORA, (L, DECAY_LORA, RWKV_WIDTH)),
        'a0': 0.1 * jax.random.normal(ks[7], (L, RWKV_WIDTH), f32),
        'a_up': dense(ks[8], AAA_LORA, (L, AAA_LORA, RWKV_WIDTH)),
        'g_up': dense(ks[9], GATE_LORA, (L, GATE_LORA, RWKV_WIDTH)),
        'k_k': 0.85 + 0.05 * jax.random.normal(ks[10], (L, RWKV_WIDTH), f32),
        'k_a': 1.0 + 0.05 * jax.random.normal(ks[11], (L, RWKV_WIDTH), f32),
        'r_k': 0.1 * jax.random.normal(ks[12], (L, RWKV_HEADS, RWKV_HEAD), f32),
        'lnx_w': gain(ks[13], (L, RWKV_WIDTH)),
        'lnx_b': 0.02 * jax.random.normal(ks[14], (L, RWKV_WIDTH), f32),
        'w_mix_out': dense(ks[15], MIX_WIDTH, (L, MIX_WIDTH, D_MODEL)),
        'norm_cross': gain(ks[16], (L, D_MODEL)),
        'norm_mem': gain(ks[17], (L, D_MODEL)),
        'w_q_cross': dense(ks[18], D_MODEL, (L, D_MODEL, CROSS_HEADS * CROSS_HEAD)),
        'w_kv_cross': dense(ks[19], D_MODEL, (L, D_MODEL, 2 * CROSS_HEADS * CROSS_HEAD)),
        'w_o_cross': dense(ks[20], CROSS_HEADS * CROSS_HEAD, (L, CROSS_HEADS * CROSS_HEAD, D_MODEL)),
        'norm_mlp': gain(ks[21], (L, D_MODEL)),
        'w_up': dense(ks[22], D_MODEL, (L, D_MODEL, D_FF)),
        'w_down': dense(ks[23], D_FF, (L, D_FF, D_MODEL)),
        'norm_final': gain(ks[24], (D_MODEL,)),
    }


def reference(x, mem, positions, norm_mix, w_in, rwkv_mu, w_decay0, w_decay_up, a0, a_up, g_up,
              k_k, k_a, r_k, lnx_w, lnx_b, w_mix_out, norm_cross, norm_mem, w_q_cross,
              w_kv_cross, w_o_cross, norm_mlp, w_up, w_down, norm_final):
    h = x
    for l in range(DEPTH):
        p = rms_norm(h, norm_mix[l]) @ w_in[l]
        y_rwkv = rwkv7_group(p[..., :RWKV_COLS], rwkv_mu[l], w_decay0[l], w_decay_up[l], a0[l],
                             a_up[l], g_up[l], k_k[l], k_a[l], r_k[l], lnx_w[l], lnx_b[l])
        y_att = dsa_group(p[..., RWKV_COLS:], positions)
        h = h + jnp.concatenate([y_rwkv, y_att], axis=-1) @ w_mix_out[l]
        h = h + memory_cross_attention(rms_norm(h, norm_cross[l]), rms_norm(mem, norm_mem[l]),
                                       w_q_cross[l], w_kv_cross[l], w_o_cross[l])
        u = rms_norm(h, norm_mlp[l]) @ w_up[l]
        h = h + jnp.square(jax.nn.relu(u)) @ w_down[l]
    return rms_norm(h, norm_final)
```

```python
import math
import os
RSTOP = int(os.environ.get('RSTOP', '99'))
PLEV = int(os.environ.get('PLEV', '7'))
PGRP = int(os.environ.get('PGRP', '8'))


class _Stop(Exception):
    pass


VAR = ''
from contextlib import ExitStack

import numpy as np
import concourse.bass as bass
import concourse.mybir as mybir
from concourse.bass_utils import run_bass_kernel_spmd

F32 = mybir.dt.float32
BF16 = mybir.dt.bfloat16
I32 = mybir.dt.int32
AF = mybir.ActivationFunctionType
ALU = mybir.AluOpType

D = 2048
SEQ = 2048
NT = 16
NMEM = 256
DFF = 8192
EPS = 1e-5
C0 = math.exp(-0.5)
NEG = -1.0e30
TWO_PI = 6.28318


class Sched:
    ENG = {"pe": "tensor", "act": "scalar", "dve": "vector", "pool": "gpsimd", "sp": "sync"}
    KD = 6
    LIMIT = 30000

    def __init__(self, nc, es):
        self.nc, self.es = nc, es
        self.h = {k: getattr(nc, v) for k, v in self.ENG.items()}
        self.semh = {}
        self.cur = {}
        self.cnt = {}
        self.nsem = 0
        for e in self.ENG:
            self._newsem(e)
        self.dsem = {}
        self.dcnt = {}
        self.dst = {}
        for q in ("sp", "pool", "act"):
            self.dsem[q] = [self._mk("d_%s%d" % (q, i)) for i in range(self.KD)]
            self.dcnt[q] = 0
            self.dst[q] = [None] * self.KD
        self.waited = {}
        self.lastw = {}
        self.readers = {}
        self.alldma = []

    def _mk(self, name):
        s = self.es.enter_context(self.nc.semaphore(name))
        self.semh[name] = s
        return name

    def _newsem(self, e):
        self.nsem += 1
        self.cur[e] = self._mk("c_%s%d" % (e, self.nsem))
        self.cnt[e] = 0

    def _deps(self, r, w):
        deps = set()
        for k in r:
            s = self.lastw.get(k)
            if s:
                deps.add(s)
        for k in w:
            s = self.lastw.get(k)
            if s:
                deps.add(s)
            for s in self.readers.get(k, ()):
                deps.add(s)
        return deps

    def _waits(self, eng, deps):
        for (name, val) in deps:
            if eng == "pe" and name == self.cur["pe"]:
                continue
            key = (eng, name)
            if self.waited.get(key, 0) < val:
                self.h[eng].wait_ge(self.semh[name], val)
                self.waited[key] = val

    def _mark(self, stamp, r, w):
        for k in w:
            self.lastw[k] = stamp
            self.readers[k] = []
        for k in r:
            self.readers.setdefault(k, []).append(stamp)

    def op(self, eng, fn, r=(), w=()):
        self._waits(eng, self._deps(r, w))
        if self.cnt[eng] >= self.LIMIT:
            self._newsem(eng)
        self.cnt[eng] += 1
        name = self.cur[eng]
        fn(self.h[eng]).then_inc(self.semh[name], 1)
        self._mark((name, self.cnt[eng]), r, w)

    def dma(self, q, out, in_, r=(), w=(), **kw):
        i = self.dcnt[q]
        slot = i % self.KD
        deps = self._deps(r, w)
        if self.dst[q][slot]:
            deps.add(self.dst[q][slot])
        self._waits(q, deps)
        name = self.dsem[q][slot]
        val = 16 * (i // self.KD + 1)
        self.h[q].dma_start(out=out, in_=in_, **kw).then_inc(self.semh[name], 16)
        stamp = (name, val)
        self.dst[q][slot] = stamp
        self.dcnt[q] += 1
        self._mark(stamp, r, w)

    def barrier(self):
        stamps = set()
        for e in self.ENG:
            if self.cnt[e] > 0:
                stamps.add((self.cur[e], self.cnt[e]))
        for q in self.dst:
            for s in self.dst[q]:
                if s:
                    stamps.add(s)
        for e in self.ENG:
            self._waits(e, stamps)
        self.lastw = {}
        self.readers = {}


def ACT(S, out, in_, func, r, w, **kw):
    S.op("act", lambda e: e.activation(out=out, in_=in_, func=func, **kw), r, w)


def TT(S, eng, out, in0, in1, op, r, w):
    S.op(eng, lambda e: e.tensor_tensor(out=out, in0=in0, in1=in1, op=op), r, w)


def TS(S, eng, out, in0, s1, s2, op0, op1, r, w):
    if op1 is None:
        S.op(eng, lambda e: e.tensor_scalar(out=out, in0=in0, scalar1=s1, scalar2=None, op0=op0), r, w)
    else:
        S.op(eng, lambda e: e.tensor_scalar(out=out, in0=in0, scalar1=s1, scalar2=s2, op0=op0, op1=op1), r, w)


def STT(S, out, in0, scalar, in1, op0, op1, r, w):
    S.op("dve", lambda e: e.scalar_tensor_tensor(out=out, in0=in0, scalar=scalar, in1=in1, op0=op0, op1=op1), r, w)


def CP(S, eng, out, in_, r, w):
    if eng == "act":
        S.op("act", lambda e: e.activation(out=out, in_=in_, func=AF.Copy), r, w)
    else:
        S.op(eng, lambda e: e.tensor_copy(out=out, in_=in_), r, w)


def MM(S, out, lhsT, rhs, start, stop, r, w):
    S.op("pe", lambda e: e.matmul(out, lhsT=lhsT, rhs=rhs, start=start, stop=stop), r, w)


def TR(S, out, in_, ident, r, w):
    S.op("pe", lambda e: e.transpose(out=out, in_=in_, identity=ident), r, w)


COLS = {}
_UN = [0]


def UN():
    _UN[0] += 1
    return "s%d_" % _UN[0]


def _col_layout():
    off = 0
    for name, n in [("g_mix", 16), ("g_cross", 16), ("g_mlp", 16), ("g_final", 16), ("g_mem", 16),
                    ("mu_rkv", 24), ("mu_xw", 1), ("mu_xa", 1), ("mu_xg", 2),
                    ("w0", 8), ("a0", 8), ("k_k", 8), ("k_a", 8), ("r_k", 8), ("lnx_w", 8), ("lnx_b", 8),
                    ("invf128", 1), ("invf64", 1)]:
        COLS[name] = (off, n)
        off += n
    return off


NCOL = _col_layout()


def _pack_cols(inp):
    a = np.zeros((128, NCOL), np.float32)

    def put(name, vec):
        o, n = COLS[name]
        v = np.zeros(n * 128, np.float32)
        v[: vec.size] = vec.reshape(-1)
        a[:, o:o + n] = v.reshape(n, 128).T

    put("g_mix", inp["norm_mix"][0]); put("g_cross", inp["norm_cross"][0]); put("g_mlp", inp["norm_mlp"][0])
    put("g_final", inp["norm_final"]); put("g_mem", inp["norm_mem"][0])
    mu = inp["rwkv_mu"][0]
    put("mu_rkv", mu[:3072]); put("mu_xw", mu[3072:3136]); put("mu_xa", mu[3136:3200]); put("mu_xg", mu[3200:3360])
    for k_ in ("w_decay0", "a0", "k_k", "k_a", "r_k", "lnx_w", "lnx_b"):
        put({"w_decay0": "w0"}.get(k_, k_), inp[k_][0])
    f128 = np.zeros(128, np.float32)
    fr = (500000.0 ** (-np.arange(16, dtype=np.float32) / 16)).astype(np.float32)
    f128[0:16] = -fr; f128[16:32] = fr
    f64 = np.zeros(128, np.float32)
    fr8 = (500000.0 ** (-np.arange(8, dtype=np.float32) / 8)).astype(np.float32)
    for b0 in (0, 64):
        f64[b0:b0 + 8] = -fr8; f64[b0 + 8:b0 + 16] = fr8
    put("invf128", f128 / (2 * np.pi)); put("invf64", f64 / (2 * np.pi))
    return a


def _consts():
    c = {}
    c["ident"] = np.eye(128, dtype=np.float32)
    bo = np.zeros((128, 128), np.float32); bo[:64, :64] = 1; bo[64:, 64:] = 1
    c["blk"] = bo
    i = np.arange(128)
    c["m_su"] = (i[:, None] < i[None, :]).astype(np.float32)
    c["m_sl"] = (i[:, None] > i[None, :]).astype(np.float32)
    c["m_u"] = (i[:, None] <= i[None, :]).astype(np.float32)
    c["cm"] = np.where(i[None, :] > i[:, None], NEG, 0.0).astype(np.float32)
    p128 = np.zeros((128, 128), np.float32)
    for m in range(16):
        p128[m + 16, m] = 1; p128[m, m + 16] = 1
    p64 = np.zeros((128, 128), np.float32)
    for b0 in (0, 64):
        for m in range(8):
            p64[b0 + m + 8, b0 + m] = 1; p64[b0 + m, b0 + m + 8] = 1
    c["p128"] = p128; c["p64"] = p64
    return np.concatenate([c[k] for k in ("ident", "blk", "m_su", "m_sl", "m_u", "cm", "p128", "p64")], axis=1)


CONST_NAMES = ("ident", "blk", "m_su", "m_sl", "m_u", "cm", "p128", "p64")


def build(debug=False, phases=None):
    phases = set(PHASES if phases is None else phases)
    nc = bass.Bass("TRN2", target_bir_lowering=False)
    es = ExitStack()
    dt_in = lambda name, shape, dt=F32: nc.dram_tensor(name, list(shape), dt, kind="ExternalInput").ap()
    x = dt_in("x", [SEQ, D]); mem = dt_in("mem", [NMEM, D]); pos = dt_in("pos", [128, SEQ], I32)
    w_in = dt_in("w_in", [D, 6000]); w_du = dt_in("w_decay_up", [64, 1024]); a_up = dt_in("a_up", [64, 1024])
    g_up = dt_in("g_up", [160, 1024]); w_mix = dt_in("w_mix_out", [D, D]); w_qc = dt_in("w_q_cross", [D, 512])
    w_kvc = dt_in("w_kv_cross", [D, 1024]); w_oc = dt_in("w_o_cross", [512, D]); w_up = dt_in("w_up", [D, DFF])
    w_dn = dt_in("w_down", [DFF, D]); colsd = dt_in("cols", [128, NCOL]); constd = dt_in("consts", [128, 1024])
    out = nc.dram_tensor("out", [SEQ, D], F32, kind="ExternalOutput").ap()
    kind = dict(kind="ExternalOutput") if debug else {}
    HT = nc.dram_tensor("HT", [16, 128, SEQ], F32, **kind).ap()
    MIXT = nc.dram_tensor("MIXT", [16, 128, SEQ], BF16, **kind).ap()

    S = Sched(nc, es)
    sb = lambda name, shape, dt: es.enter_context(nc.sbuf_tensor(UN() + name, list(shape), dt))
    PS = es.enter_context(nc.psum_tensor("ps", [128, 4096], F32))
    PSB = PS[:, :].bitcast(BF16)
    bank = lambda b: [("ps", b)]

    cols = sb("cols", [128, NCOL], F32)
    cst = sb("cst", [128, 1024], F32)
    cstb = sb("cstb", [128, 1024], BF16)
    omm = sb("omm", [128, 28], F32)
    onesb = sb("onesb", [128, 128], BF16)
    onesf = sb("onesf", [128, 128], F32)
    S.dma("sp", cols[:], colsd[:, :], w=["cols"])
    S.dma("sp", cst[:], constd[:, :], w=["cst"])
    CP(S, "dve", cstb[:], cst[:], ["cst"], ["cstb"])
    S.op("dve", lambda e: e.memset(onesb[:], 1.0), w=["onesb"])
    S.op("dve", lambda e: e.memset(onesf[:], 1.0 / D), w=["onesf"])
    o_mu = COLS["mu_rkv"][0]
    TS(S, "dve", omm[:], cols[:, o_mu:o_mu + 28], -1.0, 1.0, ALU.mult, ALU.add, ["cols"], ["omm"])
    CI = {n: i * 128 for i, n in enumerate(CONST_NAMES)}
    cf = lambda n: cst[:, CI[n]:CI[n] + 128]
    cb = lambda n: cstb[:, CI[n]:CI[n] + 128]
    col = lambda name, j=0: cols[:, COLS[name][0] + j:COLS[name][0] + j + 1]

    XT = sb("XT", [128, 16, SEQ], BF16)
    WB = [sb("wb%d" % i, [128, 16, 128], BF16) for i in range(2)]
    st = {"wslot": 0, "acc": 0}

    def load_w(w_ap, kch, m, dup64=False):
        slot = st["wslot"]; st["wslot"] = (slot + 1) % 2
        wb = WB[slot]
        rows = w_ap.shape[0]
        if rows >= 128:
            src = w_ap.rearrange("(c p) m -> p c m", p=128)
            S.dma("pool", wb[:, 0:kch, 0:m], src, w=[("wb", slot)])
            if dup64:
                S.dma("pool", wb[:, 0:kch, 64:64 + m], src, w=[("wb", slot)])
        else:
            S.dma("pool", wb[0:rows, 0, 0:m], w_ap, w=[("wb", slot)])
        return wb, slot

    def proj_fm(Xf, xkey, kparts, w_ap, m, epilogue, ntok=SEQ, dup64=False):
        kch = len(kparts)
        wb, slot = load_w(w_ap, kch, m, dup64)
        mm_ = 128 if dup64 else m
        a = st["acc"]; st["acc"] ^= 1
        base = a * 2048
        ng = (ntok + 511) // 512
        for c in range(kch):
            kp = kparts[c]
            for tg in range(ng):
                n = min(512, ntok - tg * 512)
                MM(S, PS[0:mm_, base + tg * 512: base + tg * 512 + n], wb[0:kp, c, 0:mm_],
                   Xf(c)[0:kp, tg * 512: tg * 512 + n], c == 0, c == kch - 1,
                   [("wb", slot)] + xkey(c), bank(a * 4 + tg))
        epilogue(PS[0:mm_, base:base + ntok], [("ps", a * 4 + g) for g in range(ng)])

    XTf = lambda c: XT[:, c, :]
    XTk = lambda c: [("XT", c)]
    K16 = [128] * 16

    def rms_fm(gname, dest):
        with nc.sbuf_tensor(UN() + "rs", [128, 2, SEQ], F32) as sq:
            a = st["acc"]; st["acc"] ^= 1
            base = a * 2048
            for n in range(16):
                b = n % 2
                S.dma("sp", HR[b][:], HT[n], r=[("HT", n)], w=[("hr", b)])
                ACT(S, sq[:, b, :], HR[b][:], AF.Square, [("hr", b)], [("sq", b)])
                for tg in range(4):
                    MM(S, PS[:, base + tg * 512: base + (tg + 1) * 512], onesf[:], sq[:, b, tg * 512:(tg + 1) * 512],
                       n == 0, n == 15, [("sq", b), "onesf"], bank(a * 4 + tg))
            kb = [("ps", a * 4 + g) for g in range(4)]
            ACT(S, RSTDB[:], PS[:, base:base + 2048], AF.Sqrt, kb, ["rstdb"], bias=EPSB[:, 0:1])
            S.op("dve", lambda e: e.reciprocal(out=RSTDB[:], in_=RSTDB[:]), ["rstdb"], ["rstdb"])
            for n in range(16):
                b = n % 2
                S.dma("sp", HR[b][:], HT[n], r=[("HT", n)], w=[("hr", b)])
                STT(S, sq[:, b, :], HR[b][:], col(gname, n), RSTDB[:], ALU.mult, ALU.mult,
                    [("hr", b), "rstdb", "cols"], [("sq", b)])
                dest(n, sq[:, b, :], [("sq", b)])

    EPSB = sb("epsb", [128, 2], F32)
    S.op("dve", lambda e: e.memset(EPSB[:, 0:1], EPS), w=["epsb"])
    S.op("dve", lambda e: e.memset(EPSB[:, 1:2], 64e-5), w=["epsb"])

    def resid_epi(n):
        def epi(ps, kb):
            hr = HR[n % 2]
            S.dma("sp", hr[:], HT[n], r=[("HT", n)], w=[("hr", n % 2)])
            TT(S, "dve", hr[:], hr[:], ps, ALU.add, [("hr", n % 2)] + kb, [("hr", n % 2)])
            S.dma("sp", HT[n], hr[:], r=[("hr", n % 2)], w=[("HT", n)])
        return epi


    with ExitStack() as p0:
        t0 = lambda name, shape, dt: p0.enter_context(nc.sbuf_tensor(UN() + name, list(shape), dt))
        xt = [t0("xt%d" % i, [128, D], F32) for i in range(2)]
        xs = [t0("xs%d" % i, [128, D], F32) for i in range(2)]
        junk = t0("junk", [128, D], BF16)
        ss = t0("ss", [128, 16], F32)
        htb = [t0("htb%d" % i, [128, 16, 128], F32) for i in range(2)]
        for tt in range(0 if VAR == 'V1' else NT):
            b = tt % 2
            tsl = slice(tt * 128, (tt + 1) * 128)
            S.dma("sp", xt[b][:], x[tsl, :], w=[("xt", b)])
            ACT(S, junk[:], xt[b][:], AF.Square, [("xt", b)], ["junk", ("ss", tt)], accum_out=ss[:, tt:tt + 1])
            ACT(S, ss[:, tt:tt + 1], ss[:, tt:tt + 1], AF.Sqrt, [("ss", tt)], [("ss", tt)], scale=1.0 / D, bias=EPSB[:, 0:1])
            S.op("dve", lambda e: e.reciprocal(out=ss[:, tt:tt + 1], in_=ss[:, tt:tt + 1]), [("ss", tt)], [("ss", tt)])
            ACT(S, xs[b][:], xt[b][:], AF.Copy, [("xt", b), ("ss", tt)], [("xs", b)], scale=ss[:, tt:tt + 1])
            for fg in range(0 if VAR == 'V3' else 4):
                bk = fg
                for j in range(4):
                    f = fg * 4 + j
                    TR(S, PS[:, bk * 512 + j * 128: bk * 512 + (j + 1) * 128], xs[b][:, f * 128:(f + 1) * 128], cf("ident"),
                       [("xs", b), "cst"], bank(bk))
                for j in range(4):
                    f = fg * 4 + j
                    src = PS[:, bk * 512 + j * 128: bk * 512 + (j + 1) * 128]
                    if True:
                        TS(S, "dve", XT[:, f, tsl], src, col("g_mix", f), None, ALU.mult, None, bank(bk) + ["cols"], [("XT", f)])
                    else:
                        ACT(S, XT[:, f, tsl], src, AF.Copy, bank(bk) + ["cols"], [("XT", f)], scale=col("g_mix", f))
            for fg in range(0 if VAR == 'V2' else 4):
                bk = 4 + fg
                for j in range(4):
                    f = fg * 4 + j
                    TR(S, PS[:, bk * 512 + j * 128: bk * 512 + (j + 1) * 128], xt[b][:, f * 128:(f + 1) * 128], cf("ident"),
                       [("xt", b), "cst"], bank(bk))
                dst = htb[b][:, fg * 4:(fg + 1) * 4, :]
                srcp = PS[:, bk * 512:(bk + 1) * 512]
                if fg % 2 == 0:
                    S.op("dve", lambda e, d_=dst, s_=srcp: e.tensor_copy(out=d_.rearrange("p a b -> p (a b)"), in_=s_), bank(bk), [("htb", b)])
                else:
                    S.op("act", lambda e, d_=dst, s_=srcp: e.activation(out=d_.rearrange("p a b -> p (a b)"), in_=s_, func=AF.Copy), bank(bk), [("htb", b)])
            if VAR != 'V2':
                S.dma("sp", HT[:, :, tsl].rearrange("c p t -> p c t"), htb[b][:], r=[("htb", b)], w=["HTall"])
    S.barrier()
    MAGIC = 12582912.0
    AB = 3360
    if "dsa" in phases:
        with ExitStack() as pd:
            td = lambda name, shape, dt: pd.enter_context(nc.sbuf_tensor(UN() + name, list(shape), dt))
            QT = td("QT", [128, 8, SEQ], BF16)
            KT = td("KT", [128, 2, SEQ], BF16)
            VTM = td("VTM", [128, 16, 256], BF16)
            QIT = td("QIT", [128, 8, SEQ], BF16)
            KIT2 = td("KIT2", [128, SEQ], BF16)
            WI = td("WI", [128, 16, 16], F32)
            with ExitStack() as pa:
                ta = lambda name, shape, dt: pa.enter_context(nc.sbuf_tensor(UN() + name, list(shape), dt))
                COSF = ta("COSF", [128, SEQ], F32)
                SINF = ta("SINF", [128, SEQ], F32)
                XR = ta("XR", [128, SEQ], F32)
                T1 = ta("T1", [128, SEQ], F32)
                T2 = ta("T2", [128, SEQ], F32)

                def build_tables(invname):
                    POSI = XR[:, :].bitcast(I32)
                    S.dma("sp", POSI, pos[:, :], w=["XR"])
                    TS(S, "dve", T1[:], POSI, col(invname), None, ALU.mult, None, ["XR", "cols"], ["T1"])
                    for (off, dst, key) in ((0.0, SINF, "SINF"), (0.25, COSF, "COSF")):
                        TS(S, "dve", T2[:], T1[:], off, MAGIC, ALU.add, ALU.add, ["T1"], ["T2"])
                        TS(S, "dve", T2[:], T2[:], -MAGIC, None, ALU.add, None, ["T2"], ["T2"])
                        STT(S, T2[:], T1[:], off, T2[:], ALU.add, ALU.subtract, ["T1", "T2"], ["T2"])
                        ACT(S, dst[:], T2[:], AF.Sin, ["T2"], [key], scale=TWO_PI)

                def rope_epi(perm, dest, dkey):
                    def epi(ps, kb):
                        CP(S, "act", XR[:], ps, kb, ["XR"])
                        o = st["acc"]
                        for tg in range(4):
                            MM(S, PS[:, o * 2048 + tg * 512: o * 2048 + (tg + 1) * 512], cf(perm), XR[:, tg * 512:(tg + 1) * 512],
                               True, True, ["cst", "XR"], bank(o * 4 + tg))
                        TT(S, "pool", T1[:], XR[:], COSF[:], ALU.mult, ["XR", "COSF"], ["T1"])
                        TT(S, "dve", T2[:], PS[:, o * 2048:(o + 1) * 2048], SINF[:], ALU.mult,
                           [("ps", o * 4 + g) for g in range(4)] + ["SINF"], ["T2"])
                        TT(S, "pool", dest, T1[:], T2[:], ALU.add, ["T1", "T2"], [dkey])
                    return epi

                build_tables("invf128")
                for h in range(8):
                    proj_fm(XTf, XTk, K16, w_in[:, AB + h * 128: AB + (h + 1) * 128], 128, rope_epi("p128", QT[:, h, :], ("QT", h)))
                for g in range(2):
                    proj_fm(XTf, XTk, K16, w_in[:, AB + 1024 + g * 128: AB + 1024 + (g + 1) * 128], 128,
                            rope_epi("p128", KT[:, g, :], ("KT", g)))
                for g in range(2):
                    def vepi(ps, kb, g=g):
                        CP(S, "act", XR[:], ps, kb, ["XR"])
                        o = st["acc"]
                        for q4 in range(4):
                            bk = o * 4 + q4
                            for j in range(4):
                                tt = q4 * 4 + j
                                TR(S, PS[:, bk * 512 + j * 128: bk * 512 + (j + 1) * 128], XR[:, tt * 128:(tt + 1) * 128], cf("ident"),
                                   ["XR", "cst"], bank(bk))
                            S.op("dve", lambda e, q4=q4, bk=bk: e.tensor_copy(
                                out=VTM[:, q4 * 4:(q4 + 1) * 4, g * 128:(g + 1) * 128],
                                in_=PS[:, bk * 512:(bk + 1) * 512].rearrange("p (a b) -> p a b", a=4)), bank(bk), ["VTM"])
                    proj_fm(XTf, XTk, K16, w_in[:, AB + 1280 + g * 128: AB + 1280 + (g + 1) * 128], 128, vepi)
                build_tables("invf64")
                for c in range(8):
                    proj_fm(XTf, XTk, K16, w_in[:, AB + 1536 + c * 128: AB + 1536 + (c + 1) * 128], 128,
                            rope_epi("p64", QIT[:, c, :], ("QIT", c)))
                proj_fm(XTf, XTk, K16, w_in[:, AB + 2560: AB + 2624], 64, rope_epi("p64", KIT2[:], "KIT2"), dup64=True)

                def wepi(ps, kb):
                    ACT(S, XR[0:16, :], ps, AF.Copy, kb, ["XR"], scale=1.0 / 32.0)
                    o = st["acc"]
                    bk = o * 4
                    for tt in range(16):
                        TR(S, PS[:, bk * 512 + tt * 16: bk * 512 + (tt + 1) * 16], XR[0:16, tt * 128:(tt + 1) * 128],
                           cst[0:16, CI["ident"]:CI["ident"] + 16], ["XR", "cst"], bank(bk))
                    S.op("dve", lambda e: e.tensor_copy(out=WI[:, :, :].rearrange("p a b -> p (a b)"), in_=PS[:, bk * 512: bk * 512 + 256]),
                         bank(bk), ["WI"])
                proj_fm(XTf, XTk, K16, w_in[:, AB + 2624: AB + 2640], 16, wepi)
            S.barrier()

            with ExitStack() as pb:
                tb = lambda name, shape, dt: pb.enter_context(nc.sbuf_tensor(UN() + name, list(shape), dt))
                ACC = tb("ACC", [128, SEQ], F32)
                WRK = tb("WRK", [128, SEQ], F32)
                RLU = [tb("RLU%d" % i, [128, 512], F32) for i in range(2)]
                M8 = tb("M8", [128, 8], F32)
                MK = tb("MK", [128, SEQ], F32)
                MKT = tb("MKT", [128, 16, 128], BF16)
                EX = [tb("EX%d" % i, [128, 512], BF16) for i in range(2)]
                PM = [tb("PM%d" % i, [128, 512], BF16) for i in range(2)]
                RDN = tb("RDN", [128, 512], F32)
                YA = [tb("YA%d" % i, [128, 512], BF16) for i in range(2)]
                SCALE = 128.0 ** -0.5
                sc_i = 0
                at_i = 0
                for bi in range(NT):
                    nk = (bi + 1) * 128
                    qs = slice(bi * 128, (bi + 1) * 128)
                    ngr = (nk + 511) // 512
                    for hh in range(16):
                        c, half = hh // 2, hh % 2
                        prt = slice(half * 64, half * 64 + 64)
                        for gq in range(ngr):
                            n = min(512, nk - gq * 512)
                            cs = slice(gq * 512, gq * 512 + n)
                            bk = sc_i % 4; rb = sc_i % 2; sc_i += 1
                            MM(S, PS[:, bk * 512: bk * 512 + n], QIT[prt, c, qs], KIT2[prt, cs], True, True,
                               [("QIT", c), "KIT2"], bank(bk))
                            ACT(S, RLU[rb][:, 0:n], PS[:, bk * 512: bk * 512 + n], AF.Relu, bank(bk), [("RLU", rb)])
                            if hh == 0:
                                TS(S, "dve", ACC[:, cs], RLU[rb][:, 0:n], WI[:, bi, hh:hh + 1], None, ALU.mult, None,
                                   [("RLU", rb), "WI"], [("ACC", gq)])
                            else:
                                STT(S, ACC[:, cs], RLU[rb][:, 0:n], WI[:, bi, hh:hh + 1], ACC[:, cs], ALU.mult, ALU.add,
                                    [("RLU", rb), "WI", ("ACC", gq)], [("ACC", gq)])
                    acck = [("ACC", gq) for gq in range(ngr)]
                    TT(S, "dve", ACC[:, qs], ACC[:, qs], cf("cm"), ALU.add, acck + ["cst"], acck)
                    if bi >= 2:
                        src = ACC
                        for rnd in range(32):
                            S.op("dve", lambda e, src=src: e.max(out=M8[:], in_=src[:, 0:nk]), acck + ["WRK"], ["M8"])
                            if rnd < 31:
                                S.op("dve", lambda e, src=src: e.match_replace(out=WRK[:, 0:nk], in_to_replace=M8[:],
                                                                              in_values=src[:, 0:nk], imm_value=NEG),
                                     acck + ["WRK", "M8"], ["WRK"])
                                src = WRK
                        TS(S, "dve", MK[:, 0:nk], ACC[:, 0:nk], M8[:, 7:8], None, ALU.is_ge, None, acck + ["M8"], ["MK"])
                    else:
                        TS(S, "dve", MK[:, 0:nk], ACC[:, 0:nk], -1.0e29, None, ALU.is_ge, None, acck, ["MK"])
                    for q4 in range((bi + 4) // 4):
                        bk = sc_i % 4; sc_i += 1
                        nj = min(4, bi + 1 - q4 * 4)
                        for j in range(nj):
                            sj = q4 * 4 + j
                            TR(S, PS[:, bk * 512 + j * 128: bk * 512 + (j + 1) * 128], MK[:, sj * 128:(sj + 1) * 128], cf("ident"),
                               ["MK", "cst"], bank(bk))
                        S.op("dve", lambda e, q4=q4, nj=nj, bk=bk: e.tensor_copy(
                            out=MKT[:, q4 * 4: q4 * 4 + nj, :].rearrange("p a b -> p (a b)"),
                            in_=PS[:, bk * 512: bk * 512 + nj * 128]), bank(bk), ["MKT"])
                    for g in range(2):
                        for sj in range(bi + 1):
                            bs = 4 + (at_i % 2); eb = at_i % 2; at_i += 1
                            MM(S, PS[:, bs * 512:(bs + 1) * 512], KT[:, g, sj * 128:(sj + 1) * 128], QT[:, 4 * g:4 * g + 4, qs],
                               True, True, [("KT", g)] + [("QT", 4 * g + i) for i in range(4)], bank(bs))
                            ACT(S, EX[eb][:], PS[:, bs * 512:(bs + 1) * 512], AF.Exp, bank(bs), [("EX", eb)], scale=SCALE)
                            for i in range(4):
                                TT(S, "pool", PM[eb][:, i * 128:(i + 1) * 128], EX[eb][:, i * 128:(i + 1) * 128], MKT[:, sj, :], ALU.mult,
                                   [("EX", eb), "MKT"], [("PM", eb)])
                            MM(S, PS[:, 6 * 512:7 * 512], VTM[:, sj, g * 128:(g + 1) * 128], PM[eb][:], sj == 0, sj == bi,
                               ["VTM", ("PM", eb)], bank(6))
                            MM(S, PS[:, 7 * 512:8 * 512], onesb[:], PM[eb][:], sj == 0, sj == bi, ["onesb", ("PM", eb)], bank(7))
                        S.op("dve", lambda e: e.reciprocal(out=RDN[:], in_=PS[:, 7 * 512:8 * 512]), bank(7), ["RDN"])
                        TT(S, "dve", YA[g][:], PS[:, 6 * 512:7 * 512], RDN[:], ALU.mult, bank(6) + ["RDN"], [("YA", g)])
                        S.dma("sp", MIXT[8 + 4 * g: 12 + 4 * g, :, qs].rearrange("h d t -> d h t"),
                              YA[g][:].rearrange("p (h t) -> p h t", h=4), r=[("YA", g)], w=[("MIXT", 8 + 4 * g)])
        S.barrier()

    if "rwkv" in phases:
        with ExitStack() as pr:
            tr_ = lambda name, shape, dt: pr.enter_context(nc.sbuf_tensor(UN() + name, list(shape), dt))
            TXW = tr_("TXW", [64, SEQ], BF16)
            XA = tr_("XA", [64, SEQ], BF16)
            SXG = tr_("SXG", [128, 2, SEQ], BF16)
            WDU = tr_("WDU", [64, 128], BF16)
            AUP = tr_("AUP", [64, 128], BF16)
            GUP = tr_("GUP", [128, 2, 128], BF16)
            FR, FK, FV, FA, FKM, F1, F2, F3 = [tr_("F%d" % i, [128, SEQ], F32) for i in range(8)]
            RT = tr_("RT", [128, SEQ], BF16)
            BH = tr_("BH", [128, SEQ], BF16)
            KH = tr_("KH", [128, SEQ], BF16)
            AT_ = tr_("ATl", [128, SEQ], BF16)
            VTM2 = tr_("VTM2", [128, 16, 128], BF16)
            BBT = tr_("BBT", [128, 16, 128], BF16)
            KBT = tr_("KBT", [128, 16, 128], BF16)
            PCOL = tr_("PCOL", [128, 16], F32)
            MS4 = tr_("MS4", [128, 4, 512], BF16)
            ONE1 = tr_("ONE1", [128, 128], F32)
            STf = tr_("STf", [128, 128], F32)
            STB = tr_("STB", [128, 128], BF16)
            MS3 = tr_("MS3", [128, 384], BF16)
            UB = tr_("UB", [128, 128], BF16)
            SAB = tr_("SAB", [128, 128], BF16)
            OUTB = RT
            WKR = tr_("WKR", [128, 4096 + 1536], BF16)
            Aw = [WKR[:, 0:512], WKR[:, 512:1024]]
            ATw = [WKR[:, 1024:1536], WKR[:, 1536:2048]]
            Tw = [WKR[:, 2048:2560], WKR[:, 2560:3072]]
            TG = [WKR[:, 3072:3584], WKR[:, 3584:4096]]
            MJ = [WKR[:, 4096:4096 + 768].rearrange("p (a b) -> p a b", a=2), WKR[:, 4096 + 768:4096 + 1536].rearrange("p (a b) -> p a b", a=2)]
            S.op("dve", lambda e: e.memset(ONE1[:], 1.0), w=["ONE1"])
            for k3, nm3 in enumerate(("m_su", "m_u", "m_u")):
                CP(S, "dve", MS3[:, k3 * 128:(k3 + 1) * 128], cf(nm3), ["cst"], ["MS3"])
            for mi_, nm in enumerate(("m_su", "m_sl", "m_u", "ident")):
                for rep in range(4):
                    CP(S, "dve", MS4[:, mi_, rep * 128:(rep + 1) * 128], cf(nm), ["cst"], ["MS4"])

            def shift_epi(m, mucol, ommcol, dst, dkey, tmp, tkey):
                def epi(ps, kb):
                    TS(S, "dve", tmp[0:m, :], ps, ommcol[0:m, :], None, ALU.mult, None, kb + ["omm"], [tkey])
                    STT(S, dst[0:m, 1:SEQ], ps[:, 0:SEQ - 1], mucol[0:m, :], tmp[0:m, 1:SEQ], ALU.mult, ALU.add,
                        kb + ["cols", tkey], [dkey])
                    CP(S, "dve", dst[0:m, 0:1], tmp[0:m, 0:1], [tkey, dkey], [dkey])
                return epi

            omc = lambda j: omm[:, j:j + 1]
            proj_fm(XTf, XTk, K16, w_in[:, 3072:3136], 64, shift_epi(64, col("mu_xw"), omc(24), F1, "F1", F2, "F2"))
            ACT(S, TXW[:], F1[0:64, :], AF.Tanh, ["F1"], ["TXW"])
            proj_fm(XTf, XTk, K16, w_in[:, 3136:3200], 64, shift_epi(64, col("mu_xa"), omc(25), F1, "F1", F2, "F2"))
            CP(S, "act", XA[:], F1[0:64, :], ["F1"], ["XA"])
            proj_fm(XTf, XTk, K16, w_in[:, 3200:3328], 128, shift_epi(128, col("mu_xg", 0), omc(26), F1, "F1", F2, "F2"))
            ACT(S, SXG[:, 0, :], F1[:], AF.Sigmoid, ["F1"], ["SXG"])
            proj_fm(XTf, XTk, K16, w_in[:, 3328:3360], 32, shift_epi(32, col("mu_xg", 1), omc(27), F1, "F1", F2, "F2"))
            ACT(S, SXG[0:32, 1, :], F1[0:32, :], AF.Sigmoid, ["F1"], ["SXG"])

            if RSTOP == 1:
                pass
            def small_mm(lhs_list, rhs_list, keys):
                a = st["acc"]; st["acc"] ^= 1
                for tg in range(4):
                    for i, (lh, rh) in enumerate(zip(lhs_list, rhs_list)):
                        MM(S, PS[:, a * 2048 + tg * 512: a * 2048 + (tg + 1) * 512], lh, rh[:, tg * 512:(tg + 1) * 512],
                           i == 0, i == len(lhs_list) - 1, keys, bank(a * 4 + tg))
                return PS[:, a * 2048:(a + 1) * 2048], [("ps", a * 4 + g) for g in range(4)]

            pbank = [0]

            def nb():
                pbank[0] = (pbank[0] + 1) % 8
                return pbank[0]

            for hp in range(int(os.environ.get('NPAIR', '8')) if RSTOP >= 6 else (1 if RSTOP > 1 else 0)):
              try:
                pcd = slice(hp * 128, (hp + 1) * 128)
                pc = slice(0, 128)
                S.dma("pool", WDU[:], w_du[:, pcd], w=["WDU"])
                S.dma("pool", AUP[:], a_up[:, pcd], w=["AUP"])
                S.dma("pool", GUP[:, 0, :], g_up[0:128, pcd], w=["GUP"])
                S.dma("pool", GUP[0:32, 1, :], g_up[128:160, pcd], w=["GUP"])
                proj_fm(XTf, XTk, K16, w_in[:, hp * 128:(hp + 1) * 128], 128, shift_epi(128, col("mu_rkv", hp), omc(hp), FR, "FR", F1, "F1"))
                proj_fm(XTf, XTk, K16, w_in[:, 1024 + hp * 128:1024 + (hp + 1) * 128], 128,
                        shift_epi(128, col("mu_rkv", 8 + hp), omc(8 + hp), FK, "FK", F1, "F1"))
                proj_fm(XTf, XTk, K16, w_in[:, 2048 + hp * 128:2048 + (hp + 1) * 128], 128,
                        shift_epi(128, col("mu_rkv", 16 + hp), omc(16 + hp), FV, "FV", F1, "F1"))
                for q4 in range(4):
                    bk = nb()
                    for j in range(4):
                        c = q4 * 4 + j
                        TR(S, PS[:, bk * 512 + j * 128: bk * 512 + (j + 1) * 128], FV[:, c * 128:(c + 1) * 128], cf("ident"), ["FV", "cst"], bank(bk))
                    CP(S, "act", VTM2[:, q4 * 4:(q4 + 1) * 4, :].rearrange("p a b -> p (a b)"), PS[:, bk * 512:(bk + 1) * 512], bank(bk), ["VTM2"])
                ps, kb = small_mm([AUP[:, pc]], [XA], ["AUP", "XA"])
                ACT(S, FA[:], ps, AF.Sigmoid, kb, ["FA"], bias=col("a0", hp))
                TS(S, "dve", F1[:], FA[:], -1.0, col("k_a", hp), ALU.add, ALU.mult, ["FA", "cols"], ["F1"])
                STT(S, FKM[:], F1[:], 1.0, FK[:], ALU.add, ALU.mult, ["F1", "FK"], ["FKM"])
                TS(S, "dve", FK[:], FK[:], col("k_k", hp), None, ALU.mult, None, ["FK", "cols"], ["FK"])
                TT(S, "pool", F1[:], FK[:], FK[:], ALU.mult, ["FK"], ["F1"])
                ps, kb = small_mm([cf("blk")], [F1], ["cst", "F1"])
                ACT(S, F2[:], ps, AF.Sqrt, kb, ["F2"])
                TS(S, "dve", F2[:], F2[:], 1e-12, None, ALU.max, None, ["F2"], ["F2"])
                S.op("dve", lambda e: e.reciprocal(out=F2[:], in_=F2[:]), ["F2"], ["F2"])
                TT(S, "pool", FK[:], FK[:], F2[:], ALU.mult, ["FK", "F2"], ["FK"])
                STT(S, F1[:], FR[:], col("r_k", hp), FKM[:], ALU.mult, ALU.mult, ["FR", "FKM", "cols"], ["F1"])
                ps, kb = small_mm([cf("blk")], [F1], ["cst", "F1"])
                TT(S, "dve", FV[:], FV[:], ps, ALU.mult, ["FV", "VTM2"] + kb, ["FV"])
                TT(S, "pool", FA[:], FA[:], FK[:], ALU.mult, ["FA", "FK"], ["FA"])
                ps, kb = small_mm([WDU[:, pc]], [TXW], ["WDU", "TXW"])
                ACT(S, F1[:], ps, AF.Sigmoid, kb, ["F1"], bias=col("w0", hp))
                if RSTOP == 2:
                    raise _Stop()
                for c in range(16):
                    cs = slice(c * 128, (c + 1) * 128)
                    S.op("dve", lambda e, cs=cs: e.tensor_tensor_scan(out=F2[:, cs], data0=ONE1[:], data1=F1[:, cs], initial=0.0,
                                                                      op0=ALU.mult, op1=ALU.add), ["F1", "ONE1", "F2"], ["F2"])
                ACT(S, F3[:], F2[:], AF.Exp, ["F2"], ["F3"], scale=-C0)
                TT(S, "pool", RT[:], FR[:], F3[:], ALU.mult, ["FR", "F3"], ["RT"])
                CP(S, "dve", PCOL[:, :], F3[:, :].rearrange("p (c t) -> p c t", t=128)[:, :, 127], ["F3"], ["PCOL"])
                TT(S, "dve", F1[:], F2[:], F1[:], ALU.subtract, ["F1", "F2"], ["F1"])
                ACT(S, F1[:], F1[:], AF.Exp, ["F1"], ["F1"], scale=-C0)
                STT(S, AT_[:], FK[:], -1.0, F1[:], ALU.mult, ALU.mult, ["FK", "F1"], ["ATl"])
                ACT(S, F3[:], F2[:], AF.Exp, ["F2", "RT", "PCOL"], ["F3"], scale=C0)
                TT(S, "pool", BH[:], FA[:], F3[:], ALU.mult, ["FA", "F3"], ["BH"])
                TT(S, "dve", KH[:], FKM[:], F3[:], ALU.mult, ["FKM", "F3"], ["KH"])
                for (srcb, dstT, dk, tmp, tk) in ((BH, BBT, "BBT", F1, "F1"), (KH, KBT, "KBT", F2, "F2")):
                    for c in range(16):
                        cs = slice(c * 128, (c + 1) * 128)
                        TS(S, "pool" if dk == "BBT" else "dve", tmp[:, cs], srcb[:, cs], PCOL[:, c:c + 1], None, ALU.mult, None,
                           [dk[0:2] if False else ("BH" if dk == "BBT" else "KH"), "PCOL", tk], [tk])
                    for q4 in range(4):
                        bk = nb()
                        for j in range(4):
                            c = q4 * 4 + j
                            TR(S, PS[:, bk * 512 + j * 128: bk * 512 + (j + 1) * 128], tmp[:, c * 128:(c + 1) * 128], cf("ident"), [tk, "cst"], bank(bk))
                        CP(S, "act", dstT[:, q4 * 4:(q4 + 1) * 4, :].rearrange("p a b -> p (a b)"), PS[:, bk * 512:(bk + 1) * 512], bank(bk), [dk])
                if RSTOP == 3:
                    raise _Stop()
                S.barrier()
                S.op("dve", lambda e: e.memset(STf[:], 0.0), w=["STf"])
                Yt = F1[:, :].rearrange("p (c v) -> p c v", v=128)
                for gq in range(PGRP):
                    bN = [nb(), nb()]
                    bNT = [nb(), nb()]
                    for ci in range(2):
                        for j in range(2):
                            c = 2 * gq + ci
                            prt = slice(j * 64, j * 64 + 64)
                            cs = slice(c * 128, (c + 1) * 128)
                            o = slice(ci * 128, (ci + 1) * 128)
                            MM(S, PS[:, bN[j] * 512:(bN[j] + 1) * 512][:, o], BH[prt, cs], AT_[prt, cs], True, True, ["BH", "ATl"], bank(bN[j]))
                            MM(S, PS[:, bNT[j] * 512:(bNT[j] + 1) * 512][:, o], AT_[prt, cs], BH[prt, cs], True, True, ["BH", "ATl"], bank(bNT[j]))
                    for j in range(2):
                        h2 = slice(j * 256, (j + 1) * 256)
                        TT(S, "dve", Aw[0][:, h2], PS[:, bN[j] * 512: bN[j] * 512 + 256], MS4[:, 0, 0:256], ALU.mult, bank(bN[j]) + ["MS4"], [("Aw", 0)])
                        TT(S, "dve", ATw[0][:, h2], PS[:, bNT[j] * 512: bNT[j] * 512 + 256], MS4[:, 1, 0:256], ALU.mult, bank(bNT[j]) + ["MS4"], [("ATw", 0)])
                    TT(S, "dve", Tw[0], Aw[0], MS4[:, 3, :], ALU.add, [("Aw", 0), "MS4"], [("Tw", 0)])
                    cur = 0
                    TGc = TG[gq % 2]
                    for lev in range(1, PLEV):
                        nxt = 1 - cur
                        last = lev == 6
                        bA, bAT, bT = nb(), nb(), nb()
                        for i in range(4):
                            o = slice(i * 128, (i + 1) * 128)
                            if not last:
                                MM(S, PS[:, bA * 512:(bA + 1) * 512][:, o], ATw[cur][:, o], Aw[cur][:, o], True, True,
                                   [("Aw", cur), ("ATw", cur)], bank(bA))
                            MM(S, PS[:, bAT * 512:(bAT + 1) * 512][:, o], Aw[cur][:, o], ATw[cur][:, o], True, True,
                               [("Aw", cur), ("ATw", cur)], bank(bAT))
                        if not last:
                            CP(S, "act", Aw[nxt], PS[:, bA * 512:(bA + 1) * 512], bank(bA), [("Aw", nxt)])
                        CP(S, "act", ATw[nxt], PS[:, bAT * 512:(bAT + 1) * 512], bank(bAT), [("ATw", nxt)])
                        for i in range(4):
                            o = slice(i * 128, (i + 1) * 128)
                            MM(S, PS[:, bT * 512:(bT + 1) * 512][:, o], ATw[nxt][:, o], Tw[cur][:, o], True, True,
                               [("ATw", nxt), ("Tw", cur)], bank(bT))
                        dstT_ = TGc if last else Tw[nxt]
                        TT(S, "dve", dstT_, PS[:, bT * 512:(bT + 1) * 512], Tw[cur], ALU.add, bank(bT) + [("Tw", cur)],
                           [("TG", gq % 2)] if last else [("Tw", nxt)])
                        cur = nxt
                    if RSTOP == 4:
                        continue
                    for c in (2 * gq, 2 * gq + 1):
                        cs = slice(c * 128, (c + 1) * 128)
                        MJc = MJ[c % 2]
                        bJ = [nb(), nb()]
                        for j in range(2):
                            prt = slice(j * 64, j * 64 + 64)
                            bb = bJ[j]
                            MM(S, PS[:, bb * 512: bb * 512 + 128], KH[prt, cs], AT_[prt, cs], True, True, ["KH", "ATl"], bank(bb))
                            MM(S, PS[:, bb * 512 + 128: bb * 512 + 256], BH[prt, cs], RT[prt, cs], True, True, ["BH", "RT"], bank(bb))
                            MM(S, PS[:, bb * 512 + 256: bb * 512 + 384], KH[prt, cs], RT[prt, cs], True, True, ["KH", "RT"], bank(bb))
                        for j in range(2):
                            TT(S, "dve", MJc[:, j, :], PS[:, bJ[j] * 512: bJ[j] * 512 + 384], MS3[:], ALU.mult, bank(bJ[j]) + ["MS3"], [("MJ", c % 2, j)])
                        bU, bS, bO, bSS = nb(), nb(), nb(), nb()
                        pu = PS[:, bU * 512: bU * 512 + 128]
                        if c > 0:
                            MM(S, pu, AT_[:, cs], STB[:], True, False, ["ATl", "STB"], bank(bU))
                        for j in range(2):
                            vo = slice(j * 64, j * 64 + 64)
                            MM(S, pu[:, vo], MJc[:, j, 0:128], VTM2[:, c, vo], c == 0, j == 1, [("MJ", c % 2, j), "VTM2"], bank(bU))
                        CP(S, "act", UB[:], pu, bank(bU), ["UB"])
                        for j in range(2):
                            vo = slice(j * 64, j * 64 + 64)
                            ti = j * 2 + (c % 2)
                            MM(S, PS[:, bS * 512: bS * 512 + 128][:, vo], TGc[:, ti * 128:(ti + 1) * 128], UB[:, vo], True, True,
                               [("TG", gq % 2), "UB"], bank(bS))
                        CP(S, "dve", SAB[:], PS[:, bS * 512: bS * 512 + 128], bank(bS), ["SAB"])
                        po = PS[:, bO * 512: bO * 512 + 128]
                        if c > 0:
                            MM(S, po, RT[:, cs], STB[:], True, False, ["RT", "STB"], bank(bO))
                        for j in range(2):
                            vo = slice(j * 64, j * 64 + 64)
                            MM(S, po[:, vo], MJc[:, j, 128:256], SAB[:, vo], c == 0, False, [("MJ", c % 2, j), "SAB"], bank(bO))
                            MM(S, po[:, vo], MJc[:, j, 256:384], VTM2[:, c, vo], False, j == 1, [("MJ", c % 2, j), "VTM2"], bank(bO))
                        CP(S, "act", Yt[:, c, :], po, bank(bO), [("Y", c)])
                        pss = PS[:, bSS * 512: bSS * 512 + 128]
                        MM(S, pss, BBT[:, c, :], SAB[:], True, False, ["BBT", "SAB"], bank(bSS))
                        MM(S, pss, KBT[:, c, :], VTM2[:, c, :], False, True, ["KBT", "VTM2"], bank(bSS))
                        for j in range(2):
                            prt = slice(j * 64, j * 64 + 64)
                            vo = slice(j * 64, j * 64 + 64)
                            if c == 0:
                                CP(S, "dve", STf[prt, vo], PS[prt, bSS * 512: bSS * 512 + 128][:, vo], bank(bSS), ["STf"])
                            else:
                                STT(S, STf[prt, vo], STf[prt, vo], PCOL[prt, c:c + 1], PS[prt, bSS * 512: bSS * 512 + 128][:, vo], ALU.mult, ALU.add,
                                    ["STf", "PCOL"] + bank(bSS), ["STf"])
                        CP(S, "pool", STB[:], STf[:], ["STf"], ["STB"])
                if RSTOP == 4:
                    raise _Stop()
                if RSTOP == 5:
                    raise _Stop()
                YT = F2
                for q4 in range(4):
                    bk = nb()
                    for j in range(4):
                        c = q4 * 4 + j
                        TR(S, PS[:, bk * 512 + j * 128: bk * 512 + (j + 1) * 128], Yt[:, c, :], cf("ident"), [("Y", c), "cst"], bank(bk))
                    CP(S, "act", YT[:, q4 * 512:(q4 + 1) * 512], PS[:, bk * 512:(bk + 1) * 512], bank(bk), ["F2"])
                ps, kb = small_mm([cf("blk")], [YT], ["cst", "F2"])
                STT(S, YT[:], ps, -1.0 / 64.0, YT[:], ALU.mult, ALU.add, kb + ["F2"], ["F2"])
                TT(S, "pool", F3[:], YT[:], YT[:], ALU.mult, ["F2"], ["F3"])
                ps, kb = small_mm([cf("blk")], [F3], ["cst", "F3"])
                ACT(S, F3[:], ps, AF.Sqrt, kb + ["F3"], ["F3"], scale=1.0 / 64.0, bias=EPSB[:, 1:2])
                S.op("dve", lambda e: e.reciprocal(out=F3[:], in_=F3[:]), ["F3"], ["F3"])
                TT(S, "dve", YT[:], YT[:], F3[:], ALU.mult, ["F2", "F3"], ["F2"])
                TS(S, "dve", YT[:], YT[:], col("lnx_w", hp), col("lnx_b", hp), ALU.mult, ALU.add, ["F2", "cols"], ["F2"])
                TT(S, "pool", YT[:], YT[:], FV[:], ALU.add, ["F2", "FV"], ["F2"])
                ps, kb = small_mm([GUP[:, 0, pc], GUP[0:32, 1, pc]], [SXG[:, 0, :], SXG[0:32, 1, :]], ["GUP", "SXG"])
                TT(S, "dve", OUTB[:], YT[:], ps, ALU.mult, ["F2"] + kb, ["RT"])
                S.dma("sp", MIXT[hp], OUTB[:], r=["RT"], w=[("MIXT", hp)])
                S.barrier()
              except _Stop:
                S.barrier()
        S.barrier()

    RSTDB = sb("rstdb", [128, SEQ], F32)
    HR = [sb("hr_%d" % i, [128, SEQ], F32) for i in range(2)]
    if "mixout" in phases:
        for c in range(16):
            S.dma("sp", XT[:, c, :], MIXT[c], w=[("XT", c)])
        for n in range(16):
            proj_fm(XTf, XTk, K16, w_mix[:, n * 128:(n + 1) * 128], 128, resid_epi(n))
        S.barrier()

    if "cross" in phases:
        with ExitStack() as pc:
            tc = lambda name, shape, dt: pc.enter_context(nc.sbuf_tensor(UN() + name, list(shape), dt))
            QC = tc("QC", [128, 4, SEQ], BF16)
            KC = tc("KC", [128, 4, NMEM], BF16)
            VC = tc("VC", [128, 2, 512], BF16)
            MT = tc("MT", [128, 16, NMEM], BF16)
            OCT = tc("OCT", [128, 4, SEQ], BF16)
            WKV = tc("WKV", [128, 16, 512], BF16)
            mtl = tc("mtl", [128, D], F32)
            msl = tc("msl", [128, D], F32)
            mjunk = tc("mjunk", [128, D], BF16)
            mss = tc("mss", [128, 2], F32)
            EE = [tc("E%d" % i, [128, 512], BF16) for i in range(2)]
            RD = tc("RD", [128, 512], F32)
            for mi in range(2):
                msl_ = slice(mi * 128, (mi + 1) * 128)
                S.dma("sp", mtl[:], mem[msl_, :], w=["mtl"])
                ACT(S, mjunk[:], mtl[:], AF.Square, ["mtl"], ["mjunk", ("mss", mi)], accum_out=mss[:, mi:mi + 1])
                ACT(S, mss[:, mi:mi + 1], mss[:, mi:mi + 1], AF.Sqrt, [("mss", mi)], [("mss", mi)], scale=1.0 / D, bias=EPSB[:, 0:1])
                S.op("dve", lambda e, mi=mi: e.reciprocal(out=mss[:, mi:mi + 1], in_=mss[:, mi:mi + 1]), [("mss", mi)], [("mss", mi)])
                ACT(S, msl[:], mtl[:], AF.Copy, ["mtl", ("mss", mi)], ["msl"], scale=mss[:, mi:mi + 1])
                for fg in range(4):
                    bk = fg
                    for j in range(4):
                        f = fg * 4 + j
                        TR(S, PS[:, bk * 512 + j * 128: bk * 512 + (j + 1) * 128], msl[:, f * 128:(f + 1) * 128], cf("ident"),
                           ["msl", "cst"], bank(bk))
                    for j in range(4):
                        f = fg * 4 + j
                        TS(S, "dve", MT[:, f, msl_], PS[:, bk * 512 + j * 128: bk * 512 + (j + 1) * 128], col("g_mem", f), None,
                           ALU.mult, None, bank(bk) + ["cols"], [("MT", f)])
            MTf = lambda c: MT[:, c, :]
            MTk = lambda c: [("MT", c)]
            for h in range(4):
                def kepi(ps, kb, h=h):
                    CP(S, "dve", KC[:, h, :], ps, kb, [("KC", h)])
                proj_fm(MTf, MTk, K16, w_kvc[:, h * 128:(h + 1) * 128], 128, kepi, ntok=NMEM)
            S.dma("pool", WKV[:], w_kvc[:, 512:1024].rearrange("(c p) m -> p c m", p=128), w=["WKV"])
            for mi in range(2):
                bk = 4 + mi
                for c in range(16):
                    MM(S, PS[:, bk * 512:(bk + 1) * 512], MT[:, c, mi * 128:(mi + 1) * 128], WKV[:, c, :], c == 0, c == 15,
                       [("MT", c), "WKV"], bank(bk))
                CP(S, "dve", VC[:, mi, :], PS[:, bk * 512:(bk + 1) * 512], bank(bk), [("VC", mi)])
            def xt_dest_c(n, src, keys):
                CP(S, "pool", XT[:, n, :], src, keys, [("XT", n)])
            rms_fm("g_cross", xt_dest_c)
            for h in range(4):
                def qepi(ps, kb, h=h):
                    CP(S, "act", QC[:, h, :], ps, kb, [("QC", h)])
                proj_fm(XTf, XTk, K16, w_qc[:, h * 128:(h + 1) * 128], 128, qepi)
            it = 0
            for h in range(4):
                for tg in range(4):
                    tsl = slice(tg * 512, (tg + 1) * 512)
                    for mi in range(2):
                        bs = 4 + (it % 2); it += 1
                        MM(S, PS[:, bs * 512:(bs + 1) * 512], KC[:, h, mi * 128:(mi + 1) * 128], QC[:, h, tsl], True, True,
                           [("KC", h), ("QC", h)], bank(bs))
                        ACT(S, EE[mi][:], PS[:, bs * 512:(bs + 1) * 512], AF.Exp, bank(bs), [("E", mi)], scale=128.0 ** -0.5)
                        MM(S, PS[:, 6 * 512:7 * 512], VC[:, mi, h * 128:(h + 1) * 128], EE[mi][:], mi == 0, mi == 1,
                           [("VC", mi), ("E", mi)], bank(6))
                        MM(S, PS[:, 7 * 512:8 * 512], onesb[:], EE[mi][:], mi == 0, mi == 1, ["onesb", ("E", mi)], bank(7))
                    S.op("dve", lambda e: e.reciprocal(out=RD[:], in_=PS[:, 7 * 512:8 * 512]), bank(7), ["RD"])
                    TT(S, "dve", OCT[:, h, tsl], PS[:, 6 * 512:7 * 512], RD[:], ALU.mult, bank(6) + ["RD"], [("OCT", h)])
            OCf = lambda c: OCT[:, c, :]
            OCk = lambda c: [("OCT", c)]
            for n in range(16):
                proj_fm(OCf, OCk, [128] * 4, w_oc[:, n * 128:(n + 1) * 128], 128, resid_epi(n))
        S.barrier()

    if "mlp" in phases:
        with ExitStack() as pm:
            AT = pm.enter_context(nc.sbuf_tensor(UN() + "AT", [128, 16, SEQ], BF16))
            RL = [pm.enter_context(nc.sbuf_tensor(UN() + "rl%d" % i, [128, SEQ], F32)) for i in range(2)]

            def xt_dest(n, src, keys):
                CP(S, "pool", XT[:, n, :], src, keys, [("XT", n)])
            rms_fm("g_mlp", xt_dest)
            ATf = lambda c: AT[:, c, :]
            ATk = lambda c: [("AT", c)]
            for gi in range(4):
                for ffc in range(16):
                    c0 = (gi * 16 + ffc) * 128

                    def epi(ps, kb, ffc=ffc):
                        b = ffc % 2
                        ACT(S, RL[b][:], ps, AF.Relu, kb, [("rl", b)])
                        TT(S, "pool", AT[:, ffc, :], RL[b][:], RL[b][:], ALU.mult, [("rl", b)], [("AT", ffc)])
                    proj_fm(XTf, XTk, K16, w_up[:, c0:c0 + 128], 128, epi)
                for n in range(16):
                    proj_fm(ATf, ATk, K16, w_dn[gi * 2048:(gi + 1) * 2048, n * 128:(n + 1) * 128], 128, resid_epi(n))
        S.barrier()

    with ExitStack() as pf:
        OT = [pf.enter_context(nc.sbuf_tensor(UN() + "ot%d" % i, [128, 16, 128], F32)) for i in range(2)]
        outv = out.rearrange("(tt p) f -> p tt f", p=128)

        def fin_dest(n, src, keys):
            b = n % 2
            for tg in range(4):
                bk = (n * 4 + tg) % 8
                for j in range(4):
                    tt = tg * 4 + j
                    TR(S, PS[:, bk * 512 + j * 128: bk * 512 + (j + 1) * 128], src[:, tt * 128:(tt + 1) * 128], cf("ident"),
                       keys + ["cst"], bank(bk))
                dst = OT[b][:, tg * 4:(tg + 1) * 4, :].rearrange("p a b -> p (a b)")
                CP(S, "act" if tg % 2 else "dve", dst, PS[:, bk * 512:(bk + 1) * 512], bank(bk), [("ot", b)])
            S.dma("sp", outv[:, :, n * 128:(n + 1) * 128], OT[b][:], r=[("ot", b)], w=[("out", n)])
        if VAR in ('V1', 'V2', 'V3'):
            S.dma("sp", out[0:128, :], HR[0][:], w=["o"])
        else:
            rms_fm("g_final", fin_dest)
    S.barrier()
    return nc


PHASES = ("dsa", "rwkv", "mixout", "cross", "mlp")


def make_in_maps(inp, nb=8):
    cols = _pack_cols(inp)
    consts = _consts()
    g = lambda k: np.ascontiguousarray(inp[k][0])
    shared = {"w_in": g("w_in"), "w_decay_up": g("w_decay_up"), "a_up": g("a_up"), "g_up": g("g_up"),
              "w_mix_out": g("w_mix_out"), "w_q_cross": g("w_q_cross"), "w_kv_cross": g("w_kv_cross"),
              "w_o_cross": g("w_o_cross"), "w_up": g("w_up"), "w_down": g("w_down"), "cols": cols, "consts": consts}
    maps = []
    for b in range(nb):
        m = dict(shared)
        m["x"] = np.ascontiguousarray(inp["x"][b]); m["mem"] = np.ascontiguousarray(inp["mem"][b])
        m["pos"] = np.ascontiguousarray(np.broadcast_to(inp["positions"][b:b + 1], (128, SEQ))).astype(np.int32)
        maps.append(m)
    return maps


def kernel(**inp):
    inp = {k: np.asarray(v) for k, v in inp.items()}
    nc = build()
    maps = make_in_maps(inp, 8)
    res = run_bass_kernel_spmd(nc, maps, core_ids=list(range(8)))
    return np.stack([np.asarray(r["out"]) for r in res.results]).astype(np.float32)
```

```python
import math
import os
RSTOP = int(os.environ.get('RSTOP', '99'))
PLEV = int(os.environ.get('PLEV', '7'))
PGRP = int(os.environ.get('PGRP', '8'))


class _Stop(Exception):
    pass


VAR = ''
from contextlib import ExitStack

import numpy as np
import concourse.bass as bass
import concourse.mybir as mybir
from concourse.bass_utils import run_bass_kernel_spmd

F32 = mybir.dt.float32
BF16 = mybir.dt.bfloat16
I32 = mybir.dt.int32
AF = mybir.ActivationFunctionType
ALU = mybir.AluOpType

D = 2048
SEQ = 2048
NT = 16
NMEM = 256
DFF = 8192
EPS = 1e-5
C0 = math.exp(-0.5)
NEG = -1.0e30
TWO_PI = 6.28318


class Sched:
    ENG = {"pe": "tensor", "act": "scalar", "dve": "vector", "pool": "gpsimd", "sp": "sync"}
    KD = 6
    LIMIT = 30000

    def __init__(self, nc, es):
        self.nc, self.es = nc, es
        self.h = {k: getattr(nc, v) for k, v in self.ENG.items()}
        self.semh = {}
        self.cur = {}
        self.cnt = {}
        self.nsem = 0
        for e in self.ENG:
            self._newsem(e)
        self.dsem = {}
        self.dcnt = {}
        self.dst = {}
        for q in ("sp", "pool", "act"):
            self.dsem[q] = [self._mk("d_%s%d" % (q, i)) for i in range(self.KD)]
            self.dcnt[q] = 0
            self.dst[q] = [None] * self.KD
        self.waited = {}
        self.lastw = {}
        self.readers = {}
        self.alldma = []

    def _mk(self, name):
        s = self.es.enter_context(self.nc.semaphore(name))
        self.semh[name] = s
        return name

    def _newsem(self, e):
        self.nsem += 1
        self.cur[e] = self._mk("c_%s%d" % (e, self.nsem))
        self.cnt[e] = 0

    def _deps(self, r, w):
        deps = set()
        for k in r:
            s = self.lastw.get(k)
            if s:
                deps.add(s)
        for k in w:
            s = self.lastw.get(k)
            if s:
                deps.add(s)
            for s in self.readers.get(k, ()):
                deps.add(s)
        return deps

    def _waits(self, eng, deps):
        for (name, val) in deps:
            if eng == "pe" and name == self.cur["pe"]:
                continue
            key = (eng, name)
            if self.waited.get(key, 0) < val:
                self.h[eng].wait_ge(self.semh[name], val)
                self.waited[key] = val

    def _mark(self, stamp, r, w):
        for k in w:
            self.lastw[k] = stamp
            self.readers[k] = []
        for k in r:
            self.readers.setdefault(k, []).append(stamp)

    def op(self, eng, fn, r=(), w=()):
        self._waits(eng, self._deps(r, w))
        if self.cnt[eng] >= self.LIMIT:
            self._newsem(eng)
        self.cnt[eng] += 1
        name = self.cur[eng]
        fn(self.h[eng]).then_inc(self.semh[name], 1)
        self._mark((name, self.cnt[eng]), r, w)

    def dma(self, q, out, in_, r=(), w=(), **kw):
        i = self.dcnt[q]
        slot = i % self.KD
        deps = self._deps(r, w)
        if self.dst[q][slot]:
            deps.add(self.dst[q][slot])
        self._waits(q, deps)
        name = self.dsem[q][slot]
        val = 16 * (i // self.KD + 1)
        self.h[q].dma_start(out=out, in_=in_, **kw).then_inc(self.semh[name], 16)
        stamp = (name, val)
        self.dst[q][slot] = stamp
        self.dcnt[q] += 1
        self._mark(stamp, r, w)

    def barrier(self):
        stamps = set()
        for e in self.ENG:
            if self.cnt[e] > 0:
                stamps.add((self.cur[e], self.cnt[e]))
        for q in self.dst:
            for s in self.dst[q]:
                if s:
                    stamps.add(s)
        for e in self.ENG:
            self._waits(e, stamps)
        self.lastw = {}
        self.readers = {}


def ACT(S, out, in_, func, r, w, **kw):
    S.op("act", lambda e: e.activation(out=out, in_=in_, func=func, **kw), r, w)


def TT(S, eng, out, in0, in1, op, r, w):
    S.op(eng, lambda e: e.tensor_tensor(out=out, in0=in0, in1=in1, op=op), r, w)


def TS(S, eng, out, in0, s1, s2, op0, op1, r, w):
    if op1 is None:
        S.op(eng, lambda e: e.tensor_scalar(out=out, in0=in0, scalar1=s1, scalar2=None, op0=op0), r, w)
    else:
        S.op(eng, lambda e: e.tensor_scalar(out=out, in0=in0, scalar1=s1, scalar2=s2, op0=op0, op1=op1), r, w)


def STT(S, out, in0, scalar, in1, op0, op1, r, w):
    S.op("dve", lambda e: e.scalar_tensor_tensor(out=out, in0=in0, scalar=scalar, in1=in1, op0=op0, op1=op1), r, w)


def CP(S, eng, out, in_, r, w):
    if eng == "act":
        S.op("act", lambda e: e.activation(out=out, in_=in_, func=AF.Copy), r, w)
    else:
        S.op(eng, lambda e: e.tensor_copy(out=out, in_=in_), r, w)


def MM(S, out, lhsT, rhs, start, stop, r, w):
    S.op("pe", lambda e: e.matmul(out, lhsT=lhsT, rhs=rhs, start=start, stop=stop), r, w)


def TR(S, out, in_, ident, r, w):
    S.op("pe", lambda e: e.transpose(out=out, in_=in_, identity=ident), r, w)


COLS = {}
_UN = [0]


def UN():
    _UN[0] += 1
    return "s%d_" % _UN[0]


def _col_layout():
    off = 0
    for name, n in [("g_mix", 16), ("g_cross", 16), ("g_mlp", 16), ("g_final", 16), ("g_mem", 16),
                    ("mu_rkv", 24), ("mu_xw", 1), ("mu_xa", 1), ("mu_xg", 2),
                    ("w0", 8), ("a0", 8), ("k_k", 8), ("k_a", 8), ("r_k", 8), ("lnx_w", 8), ("lnx_b", 8),
                    ("invf128", 1), ("invf64", 1)]:
        COLS[name] = (off, n)
        off += n
    return off


NCOL = _col_layout()


def _pack_cols(inp):
    a = np.zeros((128, NCOL), np.float32)

    def put(name, vec):
        o, n = COLS[name]
        v = np.zeros(n * 128, np.float32)
        v[: vec.size] = vec.reshape(-1)
        a[:, o:o + n] = v.reshape(n, 128).T

    put("g_mix", inp["norm_mix"][0]); put("g_cross", inp["norm_cross"][0]); put("g_mlp", inp["norm_mlp"][0])
    put("g_final", inp["norm_final"]); put("g_mem", inp["norm_mem"][0])
    mu = inp["rwkv_mu"][0]
    put("mu_rkv", mu[:3072]); put("mu_xw", mu[3072:3136]); put("mu_xa", mu[3136:3200]); put("mu_xg", mu[3200:3360])
    for k_ in ("w_decay0", "a0", "k_k", "k_a", "r_k", "lnx_w", "lnx_b"):
        put({"w_decay0": "w0"}.get(k_, k_), inp[k_][0])
    f128 = np.zeros(128, np.float32)
    fr = (500000.0 ** (-np.arange(16, dtype=np.float32) / 16)).astype(np.float32)
    f128[0:16] = -fr; f128[16:32] = fr
    f64 = np.zeros(128, np.float32)
    fr8 = (500000.0 ** (-np.arange(8, dtype=np.float32) / 8)).astype(np.float32)
    for b0 in (0, 64):
        f64[b0:b0 + 8] = -fr8; f64[b0 + 8:b0 + 16] = fr8
    put("invf128", f128 / (2 * np.pi)); put("invf64", f64 / (2 * np.pi))
    return a


def _consts():
    c = {}
    c["ident"] = np.eye(128, dtype=np.float32)
    bo = np.zeros((128, 128), np.float32); bo[:64, :64] = 1; bo[64:, 64:] = 1
    c["blk"] = bo
    i = np.arange(128)
    c["m_su"] = (i[:, None] < i[None, :]).astype(np.float32)
    c["m_sl"] = (i[:, None] > i[None, :]).astype(np.float32)
    c["m_u"] = (i[:, None] <= i[None, :]).astype(np.float32)
    c["cm"] = np.where(i[None, :] > i[:, None], NEG, 0.0).astype(np.float32)
    p128 = np.zeros((128, 128), np.float32)
    for m in range(16):
        p128[m + 16, m] = 1; p128[m, m + 16] = 1
    p64 = np.zeros((128, 128), np.float32)
    for b0 in (0, 64):
        for m in range(8):
            p64[b0 + m + 8, b0 + m] = 1; p64[b0 + m, b0 + m + 8] = 1
    c["p128"] = p128; c["p64"] = p64
    return np.concatenate([c[k] for k in ("ident", "blk", "m_su", "m_sl", "m_u", "cm", "p128", "p64")], axis=1)


CONST_NAMES = ("ident", "blk", "m_su", "m_sl", "m_u", "cm", "p128", "p64")


def build(debug=False, phases=None):
    phases = set(PHASES if phases is None else phases)
    nc = bass.Bass("TRN2", target_bir_lowering=False)
    es = ExitStack()
    dt_in = lambda name, shape, dt=F32: nc.dram_tensor(name, list(shape), dt, kind="ExternalInput").ap()
    x = dt_in("x", [SEQ, D]); mem = dt_in("mem", [NMEM, D]); pos = dt_in("pos", [128, SEQ], I32)
    w_in = dt_in("w_in", [D, 6000]); w_du = dt_in("w_decay_up", [64, 1024]); a_up = dt_in("a_up", [64, 1024])
    g_up = dt_in("g_up", [160, 1024]); w_mix = dt_in("w_mix_out", [D, D]); w_qc = dt_in("w_q_cross", [D, 512])
    w_kvc = dt_in("w_kv_cross", [D, 1024]); w_oc = dt_in("w_o_cross", [512, D]); w_up = dt_in("w_up", [D, DFF])
    w_dn = dt_in("w_down", [DFF, D]); colsd = dt_in("cols", [128, NCOL]); constd = dt_in("consts", [128, 1024])
    out = nc.dram_tensor("out", [SEQ, D], F32, kind="ExternalOutput").ap()
    kind = dict(kind="ExternalOutput") if debug else {}
    HT = nc.dram_tensor("HT", [16, 128, SEQ], F32, **kind).ap()
    MIXT = nc.dram_tensor("MIXT", [16, 128, SEQ], BF16, **kind).ap()

    S = Sched(nc, es)
    sb = lambda name, shape, dt: es.enter_context(nc.sbuf_tensor(UN() + name, list(shape), dt))
    PS = es.enter_context(nc.psum_tensor("ps", [128, 4096], F32))
    PSB = PS[:, :].bitcast(BF16)
    bank = lambda b: [("ps", b)]

    cols = sb("cols", [128, NCOL], F32)
    cst = sb("cst", [128, 1024], F32)
    cstb = sb("cstb", [128, 1024], BF16)
    omm = sb("omm", [128, 28], F32)
    onesb = sb("onesb", [128, 128], BF16)
    onesf = sb("onesf", [128, 128], F32)
    S.dma("sp", cols[:], colsd[:, :], w=["cols"])
    S.dma("sp", cst[:], constd[:, :], w=["cst"])
    CP(S, "dve", cstb[:], cst[:], ["cst"], ["cstb"])
    S.op("dve", lambda e: e.memset(onesb[:], 1.0), w=["onesb"])
    S.op("dve", lambda e: e.memset(onesf[:], 1.0 / D), w=["onesf"])
    o_mu = COLS["mu_rkv"][0]
    TS(S, "dve", omm[:], cols[:, o_mu:o_mu + 28], -1.0, 1.0, ALU.mult, ALU.add, ["cols"], ["omm"])
    CI = {n: i * 128 for i, n in enumerate(CONST_NAMES)}
    cf = lambda n: cst[:, CI[n]:CI[n] + 128]
    cb = lambda n: cstb[:, CI[n]:CI[n] + 128]
    col = lambda name, j=0: cols[:, COLS[name][0] + j:COLS[name][0] + j + 1]

    XT = sb("XT", [128, 16, SEQ], BF16)
    WB = [sb("wb%d" % i, [128, 16, 128], BF16) for i in range(2)]
    st = {"wslot": 0, "acc": 0}

    def load_w(w_ap, kch, m, dup64=False):
        slot = st["wslot"]; st["wslot"] = (slot + 1) % 2
        wb = WB[slot]
        rows = w_ap.shape[0]
        allk = [("wb", slot, g) for g in range(4)]
        if rows >= 128 and kch == 16 and not dup64:
            for g in range(4):
                src = w_ap[g * 512:(g + 1) * 512, :].rearrange("(c p) m -> p c m", p=128)
                S.dma("pool", wb[:, 4 * g:4 * g + 4, 0:m], src, w=[("wb", slot, g)])
        elif rows >= 128:
            src = w_ap.rearrange("(c p) m -> p c m", p=128)
            S.dma("pool", wb[:, 0:kch, 0:m], src, w=allk)
            if dup64:
                S.dma("pool", wb[:, 0:kch, 64:64 + m], src, w=allk)
        else:
            S.dma("pool", wb[0:rows, 0, 0:m], w_ap, w=allk)
        return wb, slot

    def proj_fm(Xf, xkey, kparts, w_ap, m, epilogue, ntok=SEQ, dup64=False):
        kch = len(kparts)
        wb, slot = load_w(w_ap, kch, m, dup64)
        mm_ = 128 if dup64 else m
        a = st["acc"]; st["acc"] ^= 1
        base = a * 2048
        ng = (ntok + 511) // 512
        for c in range(kch):
            kp = kparts[c]
            for tg in range(ng):
                n = min(512, ntok - tg * 512)
                MM(S, PS[0:mm_, base + tg * 512: base + tg * 512 + n], wb[0:kp, c, 0:mm_],
                   Xf(c)[0:kp, tg * 512: tg * 512 + n], c == 0, c == kch - 1,
                   [("wb", slot, (c // 4) if kch == 16 else 0)] + xkey(c), bank(a * 4 + tg))
        epilogue(PS[0:mm_, base:base + ntok], [("ps", a * 4 + g) for g in range(ng)])

    XTf = lambda c: XT[:, c, :]
    XTk = lambda c: [("XT", c)]
    K16 = [128] * 16

    def rms_fm(gname, dest):
        with nc.sbuf_tensor(UN() + "rs", [128, 2, SEQ], F32) as sq:
            a = st["acc"]; st["acc"] ^= 1
            base = a * 2048
            for n in range(16):
                b = n % 2
                S.dma("sp", HR[b][:], HT[n], r=[("HT", n)], w=[("hr", b)])
                ACT(S, sq[:, b, :], HR[b][:], AF.Square, [("hr", b)], [("sq", b)])
                for tg in range(4):
                    MM(S, PS[:, base + tg * 512: base + (tg + 1) * 512], onesf[:], sq[:, b, tg * 512:(tg + 1) * 512],
                       n == 0, n == 15, [("sq", b), "onesf"], bank(a * 4 + tg))
            kb = [("ps", a * 4 + g) for g in range(4)]
            ACT(S, RSTDB[:], PS[:, base:base + 2048], AF.Sqrt, kb, ["rstdb"], bias=EPSB[:, 0:1])
            S.op("dve", lambda e: e.reciprocal(out=RSTDB[:], in_=RSTDB[:]), ["rstdb"], ["rstdb"])
            for n in range(16):
                b = n % 2
                S.dma("sp", HR[b][:], HT[n], r=[("HT", n)], w=[("hr", b)])
                STT(S, sq[:, b, :], HR[b][:], col(gname, n), RSTDB[:], ALU.mult, ALU.mult,
                    [("hr", b), "rstdb", "cols"], [("sq", b)])
                dest(n, sq[:, b, :], [("sq", b)])

    EPSB = sb("epsb", [128, 2], F32)
    S.op("dve", lambda e: e.memset(EPSB[:, 0:1], EPS), w=["epsb"])
    S.op("dve", lambda e: e.memset(EPSB[:, 1:2], 64e-5), w=["epsb"])

    def resid_epi(n):
        def epi(ps, kb):
            hr = HR[n % 2]
            S.dma("sp", hr[:], HT[n], r=[("HT", n)], w=[("hr", n % 2)])
            TT(S, "dve", hr[:], hr[:], ps, ALU.add, [("hr", n % 2)] + kb, [("hr", n % 2)])
            S.dma("sp", HT[n], hr[:], r=[("hr", n % 2)], w=[("HT", n)])
        return epi


    with ExitStack() as p0:
        t0 = lambda name, shape, dt: p0.enter_context(nc.sbuf_tensor(UN() + name, list(shape), dt))
        xt = [t0("xt%d" % i, [128, D], F32) for i in range(2)]
        xs = [t0("xs%d" % i, [128, D], F32) for i in range(2)]
        junk = t0("junk", [128, D], BF16)
        ss = t0("ss", [128, 16], F32)
        htb = [t0("htb%d" % i, [128, 16, 128], F32) for i in range(2)]
        for tt in range(0 if VAR == 'V1' else NT):
            b = tt % 2
            tsl = slice(tt * 128, (tt + 1) * 128)
            S.dma("sp", xt[b][:], x[tsl, :], w=[("xt", b)])
            ACT(S, junk[:], xt[b][:], AF.Square, [("xt", b)], ["junk", ("ss", tt)], accum_out=ss[:, tt:tt + 1])
            ACT(S, ss[:, tt:tt + 1], ss[:, tt:tt + 1], AF.Sqrt, [("ss", tt)], [("ss", tt)], scale=1.0 / D, bias=EPSB[:, 0:1])
            S.op("dve", lambda e: e.reciprocal(out=ss[:, tt:tt + 1], in_=ss[:, tt:tt + 1]), [("ss", tt)], [("ss", tt)])
            ACT(S, xs[b][:], xt[b][:], AF.Copy, [("xt", b), ("ss", tt)], [("xs", b)], scale=ss[:, tt:tt + 1])
            for fg in range(0 if VAR == 'V3' else 4):
                bk = fg
                for j in range(4):
                    f = fg * 4 + j
                    TR(S, PS[:, bk * 512 + j * 128: bk * 512 + (j + 1) * 128], xs[b][:, f * 128:(f + 1) * 128], cf("ident"),
                       [("xs", b), "cst"], bank(bk))
                for j in range(4):
                    f = fg * 4 + j
                    src = PS[:, bk * 512 + j * 128: bk * 512 + (j + 1) * 128]
                    if True:
                        TS(S, "dve", XT[:, f, tsl], src, col("g_mix", f), None, ALU.mult, None, bank(bk) + ["cols"], [("XT", f)])
                    else:
                        ACT(S, XT[:, f, tsl], src, AF.Copy, bank(bk) + ["cols"], [("XT", f)], scale=col("g_mix", f))
            for fg in range(0 if VAR == 'V2' else 4):
                bk = 4 + fg
                for j in range(4):
                    f = fg * 4 + j
                    TR(S, PS[:, bk * 512 + j * 128: bk * 512 + (j + 1) * 128], xt[b][:, f * 128:(f + 1) * 128], cf("ident"),
                       [("xt", b), "cst"], bank(bk))
                dst = htb[b][:, fg * 4:(fg + 1) * 4, :]
                srcp = PS[:, bk * 512:(bk + 1) * 512]
                if fg % 2 == 0:
                    S.op("dve", lambda e, d_=dst, s_=srcp: e.tensor_copy(out=d_.rearrange("p a b -> p (a b)"), in_=s_), bank(bk), [("htb", b)])
                else:
                    S.op("act", lambda e, d_=dst, s_=srcp: e.activation(out=d_.rearrange("p a b -> p (a b)"), in_=s_, func=AF.Copy), bank(bk), [("htb", b)])
            if VAR != 'V2':
                S.dma("sp", HT[:, :, tsl].rearrange("c p t -> p c t"), htb[b][:], r=[("htb", b)], w=["HTall"])
    S.barrier()
    MAGIC = 12582912.0
    AB = 3360
    if "dsa" in phases:
        with ExitStack() as pd:
            td = lambda name, shape, dt: pd.enter_context(nc.sbuf_tensor(UN() + name, list(shape), dt))
            QT = td("QT", [128, 8, SEQ], BF16)
            KT = td("KT", [128, 2, SEQ], BF16)
            VTM = td("VTM", [128, 16, 256], BF16)
            QIT = td("QIT", [128, 8, SEQ], BF16)
            KIT2 = td("KIT2", [128, SEQ], BF16)
            WI = td("WI", [128, 16, 16], F32)
            with ExitStack() as pa:
                ta = lambda name, shape, dt: pa.enter_context(nc.sbuf_tensor(UN() + name, list(shape), dt))
                COSF = ta("COSF", [128, SEQ], F32)
                SINF = ta("SINF", [128, SEQ], F32)
                XR = ta("XR", [128, SEQ], F32)
                T1 = ta("T1", [128, SEQ], F32)
                T2 = ta("T2", [128, SEQ], F32)

                def build_tables(invname):
                    POSI = XR[:, :].bitcast(I32)
                    S.dma("sp", POSI, pos[:, :], w=["XR"])
                    TS(S, "dve", T1[:], POSI, col(invname), None, ALU.mult, None, ["XR", "cols"], ["T1"])
                    for (off, dst, key) in ((0.0, SINF, "SINF"), (0.25, COSF, "COSF")):
                        TS(S, "dve", T2[:], T1[:], off, MAGIC, ALU.add, ALU.add, ["T1"], ["T2"])
                        TS(S, "dve", T2[:], T2[:], -MAGIC, None, ALU.add, None, ["T2"], ["T2"])
                        STT(S, T2[:], T1[:], off, T2[:], ALU.add, ALU.subtract, ["T1", "T2"], ["T2"])
                        ACT(S, dst[:], T2[:], AF.Sin, ["T2"], [key], scale=TWO_PI)

                def rope_epi(perm, dest, dkey):
                    def epi(ps, kb):
                        CP(S, "act", XR[:], ps, kb, ["XR"])
                        o = st["acc"]
                        for tg in range(4):
                            MM(S, PS[:, o * 2048 + tg * 512: o * 2048 + (tg + 1) * 512], cf(perm), XR[:, tg * 512:(tg + 1) * 512],
                               True, True, ["cst", "XR"], bank(o * 4 + tg))
                        TT(S, "pool", T1[:], XR[:], COSF[:], ALU.mult, ["XR", "COSF"], ["T1"])
                        TT(S, "dve", T2[:], PS[:, o * 2048:(o + 1) * 2048], SINF[:], ALU.mult,
                           [("ps", o * 4 + g) for g in range(4)] + ["SINF"], ["T2"])
                        TT(S, "pool", dest, T1[:], T2[:], ALU.add, ["T1", "T2"], [dkey])
                    return epi

                build_tables("invf128")
                for h in range(8):
                    proj_fm(XTf, XTk, K16, w_in[:, AB + h * 128: AB + (h + 1) * 128], 128, rope_epi("p128", QT[:, h, :], ("QT", h)))
                for g in range(2):
                    proj_fm(XTf, XTk, K16, w_in[:, AB + 1024 + g * 128: AB + 1024 + (g + 1) * 128], 128,
                            rope_epi("p128", KT[:, g, :], ("KT", g)))
                for g in range(2):
                    def vepi(ps, kb, g=g):
                        CP(S, "act", XR[:], ps, kb, ["XR"])
                        o = st["acc"]
                        for q4 in range(4):
                            bk = o * 4 + q4
                            for j in range(4):
                                tt = q4 * 4 + j
                                TR(S, PS[:, bk * 512 + j * 128: bk * 512 + (j + 1) * 128], XR[:, tt * 128:(tt + 1) * 128], cf("ident"),
                                   ["XR", "cst"], bank(bk))
                            S.op("dve", lambda e, q4=q4, bk=bk: e.tensor_copy(
                                out=VTM[:, q4 * 4:(q4 + 1) * 4, g * 128:(g + 1) * 128],
                                in_=PS[:, bk * 512:(bk + 1) * 512].rearrange("p (a b) -> p a b", a=4)), bank(bk), ["VTM"])
                    proj_fm(XTf, XTk, K16, w_in[:, AB + 1280 + g * 128: AB + 1280 + (g + 1) * 128], 128, vepi)
                build_tables("invf64")
                for c in range(8):
                    proj_fm(XTf, XTk, K16, w_in[:, AB + 1536 + c * 128: AB + 1536 + (c + 1) * 128], 128,
                            rope_epi("p64", QIT[:, c, :], ("QIT", c)))
                proj_fm(XTf, XTk, K16, w_in[:, AB + 2560: AB + 2624], 64, rope_epi("p64", KIT2[:], "KIT2"), dup64=True)

                def wepi(ps, kb):
                    ACT(S, XR[0:16, :], ps, AF.Copy, kb, ["XR"], scale=1.0 / 32.0)
                    o = st["acc"]
                    bk = o * 4
                    for tt in range(16):
                        TR(S, PS[:, bk * 512 + tt * 16: bk * 512 + (tt + 1) * 16], XR[0:16, tt * 128:(tt + 1) * 128],
                           cst[0:16, CI["ident"]:CI["ident"] + 16], ["XR", "cst"], bank(bk))
                    S.op("dve", lambda e: e.tensor_copy(out=WI[:, :, :].rearrange("p a b -> p (a b)"), in_=PS[:, bk * 512: bk * 512 + 256]),
                         bank(bk), ["WI"])
                proj_fm(XTf, XTk, K16, w_in[:, AB + 2624: AB + 2640], 16, wepi)
            S.barrier()

            with ExitStack() as pb:
                tb = lambda name, shape, dt: pb.enter_context(nc.sbuf_tensor(UN() + name, list(shape), dt))
                ACC = tb("ACC", [128, SEQ], F32)
                WRK = tb("WRK", [128, SEQ], F32)
                RLU = [tb("RLU%d" % i, [128, 512], F32) for i in range(2)]
                M8 = tb("M8", [128, 8], F32)
                MK = tb("MK", [128, SEQ], F32)
                MKT = tb("MKT", [128, 16, 128], BF16)
                EX = [tb("EX%d" % i, [128, 512], BF16) for i in range(2)]
                PM = [tb("PM%d" % i, [128, 512], BF16) for i in range(2)]
                RDN = tb("RDN", [128, 512], F32)
                YA = [tb("YA%d" % i, [128, 512], BF16) for i in range(2)]
                SCALE = 128.0 ** -0.5
                sc_i = 0
                at_i = 0
                for bi in range(NT):
                    nk = (bi + 1) * 128
                    qs = slice(bi * 128, (bi + 1) * 128)
                    ngr = (nk + 511) // 512
                    for hh in range(16):
                        c, half = hh // 2, hh % 2
                        prt = slice(half * 64, half * 64 + 64)
                        for gq in range(ngr):
                            n = min(512, nk - gq * 512)
                            cs = slice(gq * 512, gq * 512 + n)
                            bk = sc_i % 4; rb = sc_i % 2; sc_i += 1
                            MM(S, PS[:, bk * 512: bk * 512 + n], QIT[prt, c, qs], KIT2[prt, cs], True, True,
                               [("QIT", c), "KIT2"], bank(bk))
                            ACT(S, RLU[rb][:, 0:n], PS[:, bk * 512: bk * 512 + n], AF.Relu, bank(bk), [("RLU", rb)])
                            if hh == 0:
                                TS(S, "dve", ACC[:, cs], RLU[rb][:, 0:n], WI[:, bi, hh:hh + 1], None, ALU.mult, None,
                                   [("RLU", rb), "WI"], [("ACC", gq)])
                            else:
                                STT(S, ACC[:, cs], RLU[rb][:, 0:n], WI[:, bi, hh:hh + 1], ACC[:, cs], ALU.mult, ALU.add,
                                    [("RLU", rb), "WI", ("ACC", gq)], [("ACC", gq)])
                    acck = [("ACC", gq) for gq in range(ngr)]
                    TT(S, "dve", ACC[:, qs], ACC[:, qs], cf("cm"), ALU.add, acck + ["cst"], acck)
                    if bi >= 2:
                        src = ACC
                        for rnd in range(32):
                            S.op("dve", lambda e, src=src: e.max(out=M8[:], in_=src[:, 0:nk]), acck + ["WRK"], ["M8"])
                            if rnd < 31:
                                S.op("dve", lambda e, src=src: e.match_replace(out=WRK[:, 0:nk], in_to_replace=M8[:],
                                                                              in_values=src[:, 0:nk], imm_value=NEG),
                                     acck + ["WRK", "M8"], ["WRK"])
                                src = WRK
                        TS(S, "dve", MK[:, 0:nk], ACC[:, 0:nk], M8[:, 7:8], None, ALU.is_ge, None, acck + ["M8"], ["MK"])
                    else:
                        TS(S, "dve", MK[:, 0:nk], ACC[:, 0:nk], -1.0e29, None, ALU.is_ge, None, acck, ["MK"])
                    for q4 in range((bi + 4) // 4):
                        bk = sc_i % 4; sc_i += 1
                        nj = min(4, bi + 1 - q4 * 4)
                        for j in range(nj):
                            sj = q4 * 4 + j
                            TR(S, PS[:, bk * 512 + j * 128: bk * 512 + (j + 1) * 128], MK[:, sj * 128:(sj + 1) * 128], cf("ident"),
                               ["MK", "cst"], bank(bk))
                        S.op("dve", lambda e, q4=q4, nj=nj, bk=bk: e.tensor_copy(
                            out=MKT[:, q4 * 4: q4 * 4 + nj, :].rearrange("p a b -> p (a b)"),
                            in_=PS[:, bk * 512: bk * 512 + nj * 128]), bank(bk), ["MKT"])
                    for g in range(2):
                        for sj in range(bi + 1):
                            bs = 4 + (at_i % 2); eb = at_i % 2; at_i += 1
                            MM(S, PS[:, bs * 512:(bs + 1) * 512], KT[:, g, sj * 128:(sj + 1) * 128], QT[:, 4 * g:4 * g + 4, qs],
                               True, True, [("KT", g)] + [("QT", 4 * g + i) for i in range(4)], bank(bs))
                            ACT(S, EX[eb][:], PS[:, bs * 512:(bs + 1) * 512], AF.Exp, bank(bs), [("EX", eb)], scale=SCALE)
                            for i in range(4):
                                TT(S, "pool", PM[eb][:, i * 128:(i + 1) * 128], EX[eb][:, i * 128:(i + 1) * 128], MKT[:, sj, :], ALU.mult,
                                   [("EX", eb), "MKT"], [("PM", eb)])
                            MM(S, PS[:, 6 * 512:7 * 512], VTM[:, sj, g * 128:(g + 1) * 128], PM[eb][:], sj == 0, sj == bi,
                               ["VTM", ("PM", eb)], bank(6))
                            MM(S, PS[:, 7 * 512:8 * 512], onesb[:], PM[eb][:], sj == 0, sj == bi, ["onesb", ("PM", eb)], bank(7))
                        S.op("dve", lambda e: e.reciprocal(out=RDN[:], in_=PS[:, 7 * 512:8 * 512]), bank(7), ["RDN"])
                        TT(S, "dve", YA[g][:], PS[:, 6 * 512:7 * 512], RDN[:], ALU.mult, bank(6) + ["RDN"], [("YA", g)])
                        S.dma("sp", MIXT[8 + 4 * g: 12 + 4 * g, :, qs].rearrange("h d t -> d h t"),
                              YA[g][:].rearrange("p (h t) -> p h t", h=4), r=[("YA", g)], w=[("MIXT", 8 + 4 * g)])
        S.barrier()

    if "rwkv" in phases:
        with ExitStack() as pr:
            tr_ = lambda name, shape, dt: pr.enter_context(nc.sbuf_tensor(UN() + name, list(shape), dt))
            TXW = tr_("TXW", [64, SEQ], BF16)
            XA = tr_("XA", [64, SEQ], BF16)
            SXG = tr_("SXG", [128, 2, SEQ], BF16)
            WDU = tr_("WDU", [64, 128], BF16)
            AUP = tr_("AUP", [64, 128], BF16)
            GUP = tr_("GUP", [128, 2, 128], BF16)
            FR, FK, FV, FA, FKM, F1, F2, F3 = [tr_("F%d" % i, [128, SEQ], F32) for i in range(8)]
            RT = tr_("RT", [128, SEQ], BF16)
            BH = tr_("BH", [128, SEQ], BF16)
            KH = tr_("KH", [128, SEQ], BF16)
            AT_ = tr_("ATl", [128, SEQ], BF16)
            VTM2 = tr_("VTM2", [128, 16, 128], BF16)
            BBT = tr_("BBT", [128, 16, 128], BF16)
            KBT = tr_("KBT", [128, 16, 128], BF16)
            PCOL = tr_("PCOL", [128, 16], F32)
            MS4 = tr_("MS4", [128, 4, 512], BF16)
            ONE1 = tr_("ONE1", [128, 128], F32)
            STf = tr_("STf", [128, 128], F32)
            STB = tr_("STB", [128, 128], BF16)
            MS3 = tr_("MS3", [128, 384], BF16)
            UB = tr_("UB", [128, 128], BF16)
            SAB = tr_("SAB", [128, 128], BF16)
            OUTB = RT
            WKR = tr_("WKR", [128, 4096 + 1536], BF16)
            Aw = [WKR[:, 0:512], WKR[:, 512:1024]]
            ATw = [WKR[:, 1024:1536], WKR[:, 1536:2048]]
            Tw = [WKR[:, 2048:2560], WKR[:, 2560:3072]]
            TG = [WKR[:, 3072:3584], WKR[:, 3584:4096]]
            MJ = [WKR[:, 4096:4096 + 768].rearrange("p (a b) -> p a b", a=2), WKR[:, 4096 + 768:4096 + 1536].rearrange("p (a b) -> p a b", a=2)]
            S.op("dve", lambda e: e.memset(ONE1[:], 1.0), w=["ONE1"])
            for k3, nm3 in enumerate(("m_su", "m_u", "m_u")):
                CP(S, "dve", MS3[:, k3 * 128:(k3 + 1) * 128], cf(nm3), ["cst"], ["MS3"])
            for mi_, nm in enumerate(("m_su", "m_sl", "m_u", "ident")):
                for rep in range(4):
                    CP(S, "dve", MS4[:, mi_, rep * 128:(rep + 1) * 128], cf(nm), ["cst"], ["MS4"])

            def shift_epi(m, mucol, ommcol, dst, dkey, tmp, tkey):
                def epi(ps, kb):
                    TS(S, "dve", tmp[0:m, :], ps, ommcol[0:m, :], None, ALU.mult, None, kb + ["omm"], [tkey])
                    STT(S, dst[0:m, 1:SEQ], ps[:, 0:SEQ - 1], mucol[0:m, :], tmp[0:m, 1:SEQ], ALU.mult, ALU.add,
                        kb + ["cols", tkey], [dkey])
                    CP(S, "dve", dst[0:m, 0:1], tmp[0:m, 0:1], [tkey, dkey], [dkey])
                return epi

            omc = lambda j: omm[:, j:j + 1]
            proj_fm(XTf, XTk, K16, w_in[:, 3072:3136], 64, shift_epi(64, col("mu_xw"), omc(24), F1, "F1", F2, "F2"))
            ACT(S, TXW[:], F1[0:64, :], AF.Tanh, ["F1"], ["TXW"])
            proj_fm(XTf, XTk, K16, w_in[:, 3136:3200], 64, shift_epi(64, col("mu_xa"), omc(25), F1, "F1", F2, "F2"))
            CP(S, "act", XA[:], F1[0:64, :], ["F1"], ["XA"])
            proj_fm(XTf, XTk, K16, w_in[:, 3200:3328], 128, shift_epi(128, col("mu_xg", 0), omc(26), F1, "F1", F2, "F2"))
            ACT(S, SXG[:, 0, :], F1[:], AF.Sigmoid, ["F1"], ["SXG"])
            proj_fm(XTf, XTk, K16, w_in[:, 3328:3360], 32, shift_epi(32, col("mu_xg", 1), omc(27), F1, "F1", F2, "F2"))
            ACT(S, SXG[0:32, 1, :], F1[0:32, :], AF.Sigmoid, ["F1"], ["SXG"])

            if RSTOP == 1:
                pass
            def small_mm(lhs_list, rhs_list, keys):
                a = st["acc"]; st["acc"] ^= 1
                for tg in range(4):
                    for i, (lh, rh) in enumerate(zip(lhs_list, rhs_list)):
                        MM(S, PS[:, a * 2048 + tg * 512: a * 2048 + (tg + 1) * 512], lh, rh[:, tg * 512:(tg + 1) * 512],
                           i == 0, i == len(lhs_list) - 1, keys, bank(a * 4 + tg))
                return PS[:, a * 2048:(a + 1) * 2048], [("ps", a * 4 + g) for g in range(4)]

            pbank = [0]

            def nb():
                pbank[0] = (pbank[0] + 1) % 8
                return pbank[0]

            for hp in range(int(os.environ.get('NPAIR', '8')) if RSTOP >= 6 else (1 if RSTOP > 1 else 0)):
              try:
                pcd = slice(hp * 128, (hp + 1) * 128)
                pc = slice(0, 128)
                S.dma("pool", WDU[:], w_du[:, pcd], w=["WDU"])
                S.dma("pool", AUP[:], a_up[:, pcd], w=["AUP"])
                S.dma("pool", GUP[:, 0, :], g_up[0:128, pcd], w=["GUP"])
                S.dma("pool", GUP[0:32, 1, :], g_up[128:160, pcd], w=["GUP"])
                proj_fm(XTf, XTk, K16, w_in[:, hp * 128:(hp + 1) * 128], 128, shift_epi(128, col("mu_rkv", hp), omc(hp), FR, "FR", F1, "F1"))
                proj_fm(XTf, XTk, K16, w_in[:, 1024 + hp * 128:1024 + (hp + 1) * 128], 128,
                        shift_epi(128, col("mu_rkv", 8 + hp), omc(8 + hp), FK, "FK", F1, "F1"))
                proj_fm(XTf, XTk, K16, w_in[:, 2048 + hp * 128:2048 + (hp + 1) * 128], 128,
                        shift_epi(128, col("mu_rkv", 16 + hp), omc(16 + hp), FV, "FV", F1, "F1"))
                for q4 in range(4):
                    bk = nb()
                    for j in range(4):
                        c = q4 * 4 + j
                        TR(S, PS[:, bk * 512 + j * 128: bk * 512 + (j + 1) * 128], FV[:, c * 128:(c + 1) * 128], cf("ident"), ["FV", "cst"], bank(bk))
                    CP(S, "act", VTM2[:, q4 * 4:(q4 + 1) * 4, :].rearrange("p a b -> p (a b)"), PS[:, bk * 512:(bk + 1) * 512], bank(bk), ["VTM2"])
                ps, kb = small_mm([AUP[:, pc]], [XA], ["AUP", "XA"])
                ACT(S, FA[:], ps, AF.Sigmoid, kb, ["FA"], bias=col("a0", hp))
                TS(S, "dve", F1[:], FA[:], -1.0, col("k_a", hp), ALU.add, ALU.mult, ["FA", "cols"], ["F1"])
                STT(S, FKM[:], F1[:], 1.0, FK[:], ALU.add, ALU.mult, ["F1", "FK"], ["FKM"])
                TS(S, "dve", FK[:], FK[:], col("k_k", hp), None, ALU.mult, None, ["FK", "cols"], ["FK"])
                TT(S, "pool", F1[:], FK[:], FK[:], ALU.mult, ["FK"], ["F1"])
                ps, kb = small_mm([cf("blk")], [F1], ["cst", "F1"])
                ACT(S, F2[:], ps, AF.Sqrt, kb, ["F2"])
                TS(S, "dve", F2[:], F2[:], 1e-12, None, ALU.max, None, ["F2"], ["F2"])
                S.op("dve", lambda e: e.reciprocal(out=F2[:], in_=F2[:]), ["F2"], ["F2"])
                TT(S, "pool", FK[:], FK[:], F2[:], ALU.mult, ["FK", "F2"], ["FK"])
                STT(S, F1[:], FR[:], col("r_k", hp), FKM[:], ALU.mult, ALU.mult, ["FR", "FKM", "cols"], ["F1"])
                ps, kb = small_mm([cf("blk")], [F1], ["cst", "F1"])
                TT(S, "dve", FV[:], FV[:], ps, ALU.mult, ["FV", "VTM2"] + kb, ["FV"])
                TT(S, "pool", FA[:], FA[:], FK[:], ALU.mult, ["FA", "FK"], ["FA"])
                ps, kb = small_mm([WDU[:, pc]], [TXW], ["WDU", "TXW"])
                ACT(S, F1[:], ps, AF.Sigmoid, kb, ["F1"], bias=col("w0", hp))
                if RSTOP == 2:
                    raise _Stop()
                for c in range(16):
                    cs = slice(c * 128, (c + 1) * 128)
                    S.op("dve", lambda e, cs=cs: e.tensor_tensor_scan(out=F2[:, cs], data0=ONE1[:], data1=F1[:, cs], initial=0.0,
                                                                      op0=ALU.mult, op1=ALU.add), ["F1", "ONE1", "F2"], ["F2"])
                ACT(S, F3[:], F2[:], AF.Exp, ["F2"], ["F3"], scale=-C0)
                TT(S, "pool", RT[:], FR[:], F3[:], ALU.mult, ["FR", "F3"], ["RT"])
                CP(S, "dve", PCOL[:, :], F3[:, :].rearrange("p (c t) -> p c t", t=128)[:, :, 127], ["F3"], ["PCOL"])
                TT(S, "dve", F1[:], F2[:], F1[:], ALU.subtract, ["F1", "F2"], ["F1"])
                ACT(S, F1[:], F1[:], AF.Exp, ["F1"], ["F1"], scale=-C0)
                STT(S, AT_[:], FK[:], -1.0, F1[:], ALU.mult, ALU.mult, ["FK", "F1"], ["ATl"])
                ACT(S, F3[:], F2[:], AF.Exp, ["F2", "RT", "PCOL"], ["F3"], scale=C0)
                TT(S, "pool", BH[:], FA[:], F3[:], ALU.mult, ["FA", "F3"], ["BH"])
                TT(S, "dve", KH[:], FKM[:], F3[:], ALU.mult, ["FKM", "F3"], ["KH"])
                for (srcb, dstT, dk, tmp, tk) in ((BH, BBT, "BBT", F1, "F1"), (KH, KBT, "KBT", F2, "F2")):
                    for c in range(16):
                        cs = slice(c * 128, (c + 1) * 128)
                        TS(S, "pool" if dk == "BBT" else "dve", tmp[:, cs], srcb[:, cs], PCOL[:, c:c + 1], None, ALU.mult, None,
                           [dk[0:2] if False else ("BH" if dk == "BBT" else "KH"), "PCOL", tk], [tk])
                    for q4 in range(4):
                        bk = nb()
                        for j in range(4):
                            c = q4 * 4 + j
                            TR(S, PS[:, bk * 512 + j * 128: bk * 512 + (j + 1) * 128], tmp[:, c * 128:(c + 1) * 128], cf("ident"), [tk, "cst"], bank(bk))
                        CP(S, "act", dstT[:, q4 * 4:(q4 + 1) * 4, :].rearrange("p a b -> p (a b)"), PS[:, bk * 512:(bk + 1) * 512], bank(bk), [dk])
                if RSTOP == 3:
                    raise _Stop()
                S.barrier()
                S.op("dve", lambda e: e.memset(STf[:], 0.0), w=["STf"])
                Yt = F1[:, :].rearrange("p (c v) -> p c v", v=128)
                for gq in range(PGRP):
                    bN = [nb(), nb()]
                    bNT = [nb(), nb()]
                    for ci in range(2):
                        for j in range(2):
                            c = 2 * gq + ci
                            prt = slice(j * 64, j * 64 + 64)
                            cs = slice(c * 128, (c + 1) * 128)
                            o = slice(ci * 128, (ci + 1) * 128)
                            MM(S, PS[:, bN[j] * 512:(bN[j] + 1) * 512][:, o], BH[prt, cs], AT_[prt, cs], True, True, ["BH", "ATl"], bank(bN[j]))
                            MM(S, PS[:, bNT[j] * 512:(bNT[j] + 1) * 512][:, o], AT_[prt, cs], BH[prt, cs], True, True, ["BH", "ATl"], bank(bNT[j]))
                    for j in range(2):
                        h2 = slice(j * 256, (j + 1) * 256)
                        TT(S, "dve", Aw[0][:, h2], PS[:, bN[j] * 512: bN[j] * 512 + 256], MS4[:, 0, 0:256], ALU.mult, bank(bN[j]) + ["MS4"], [("Aw", 0)])
                        TT(S, "dve", ATw[0][:, h2], PS[:, bNT[j] * 512: bNT[j] * 512 + 256], MS4[:, 1, 0:256], ALU.mult, bank(bNT[j]) + ["MS4"], [("ATw", 0)])
                    TT(S, "dve", Tw[0], Aw[0], MS4[:, 3, :], ALU.add, [("Aw", 0), "MS4"], [("Tw", 0)])
                    cur = 0
                    TGc = TG[gq % 2]
                    for lev in range(1, PLEV):
                        nxt = 1 - cur
                        last = lev == 6
                        bA, bAT, bT = nb(), nb(), nb()
                        for i in range(4):
                            o = slice(i * 128, (i + 1) * 128)
                            if not last:
                                MM(S, PS[:, bA * 512:(bA + 1) * 512][:, o], ATw[cur][:, o], Aw[cur][:, o], True, True,
                                   [("Aw", cur), ("ATw", cur)], bank(bA))
                            MM(S, PS[:, bAT * 512:(bAT + 1) * 512][:, o], Aw[cur][:, o], ATw[cur][:, o], True, True,
                               [("Aw", cur), ("ATw", cur)], bank(bAT))
                        if not last:
                            CP(S, "act", Aw[nxt], PS[:, bA * 512:(bA + 1) * 512], bank(bA), [("Aw", nxt)])
                        CP(S, "act", ATw[nxt], PS[:, bAT * 512:(bAT + 1) * 512], bank(bAT), [("ATw", nxt)])
                        for i in range(4):
                            o = slice(i * 128, (i + 1) * 128)
                            MM(S, PS[:, bT * 512:(bT + 1) * 512][:, o], ATw[nxt][:, o], Tw[cur][:, o], True, True,
                               [("ATw", nxt), ("Tw", cur)], bank(bT))
                        dstT_ = TGc if last else Tw[nxt]
                        TT(S, "dve", dstT_, PS[:, bT * 512:(bT + 1) * 512], Tw[cur], ALU.add, bank(bT) + [("Tw", cur)],
                           [("TG", gq % 2)] if last else [("Tw", nxt)])
                        cur = nxt
                    if RSTOP == 4:
                        continue
                    for c in (2 * gq, 2 * gq + 1):
                        cs = slice(c * 128, (c + 1) * 128)
                        MJc = MJ[c % 2]
                        bJ = [nb(), nb()]
                        for j in range(2):
                            prt = slice(j * 64, j * 64 + 64)
                            bb = bJ[j]
                            MM(S, PS[:, bb * 512: bb * 512 + 128], KH[prt, cs], AT_[prt, cs], True, True, ["KH", "ATl"], bank(bb))
                            MM(S, PS[:, bb * 512 + 128: bb * 512 + 256], BH[prt, cs], RT[prt, cs], True, True, ["BH", "RT"], bank(bb))
                            MM(S, PS[:, bb * 512 + 256: bb * 512 + 384], KH[prt, cs], RT[prt, cs], True, True, ["KH", "RT"], bank(bb))
                        for j in range(2):
                            TT(S, "dve", MJc[:, j, :], PS[:, bJ[j] * 512: bJ[j] * 512 + 384], MS3[:], ALU.mult, bank(bJ[j]) + ["MS3"], [("MJ", c % 2, j)])
                        bU, bS, bO, bSS = nb(), nb(), nb(), nb()
                        pu = PS[:, bU * 512: bU * 512 + 128]
                        if c > 0:
                            MM(S, pu, AT_[:, cs], STB[:], True, False, ["ATl", "STB"], bank(bU))
                        for j in range(2):
                            vo = slice(j * 64, j * 64 + 64)
                            MM(S, pu[:, vo], MJc[:, j, 0:128], VTM2[:, c, vo], c == 0, j == 1, [("MJ", c % 2, j), "VTM2"], bank(bU))
                        CP(S, "act", UB[:], pu, bank(bU), ["UB"])
                        for j in range(2):
                            vo = slice(j * 64, j * 64 + 64)
                            ti = j * 2 + (c % 2)
                            MM(S, PS[:, bS * 512: bS * 512 + 128][:, vo], TGc[:, ti * 128:(ti + 1) * 128], UB[:, vo], True, True,
                               [("TG", gq % 2), "UB"], bank(bS))
                        CP(S, "dve", SAB[:], PS[:, bS * 512: bS * 512 + 128], bank(bS), ["SAB"])
                        po = PS[:, bO * 512: bO * 512 + 128]
                        if c > 0:
                            MM(S, po, RT[:, cs], STB[:], True, False, ["RT", "STB"], bank(bO))
                        for j in range(2):
                            vo = slice(j * 64, j * 64 + 64)
                            MM(S, po[:, vo], MJc[:, j, 128:256], SAB[:, vo], c == 0, False, [("MJ", c % 2, j), "SAB"], bank(bO))
                            MM(S, po[:, vo], MJc[:, j, 256:384], VTM2[:, c, vo], False, j == 1, [("MJ", c % 2, j), "VTM2"], bank(bO))
                        CP(S, "act", Yt[:, c, :], po, bank(bO), [("Y", c)])
                        pss = PS[:, bSS * 512: bSS * 512 + 128]
                        MM(S, pss, BBT[:, c, :], SAB[:], True, False, ["BBT", "SAB"], bank(bSS))
                        MM(S, pss, KBT[:, c, :], VTM2[:, c, :], False, True, ["KBT", "VTM2"], bank(bSS))
                        for j in range(2):
                            prt = slice(j * 64, j * 64 + 64)
                            vo = slice(j * 64, j * 64 + 64)
                            if c == 0:
                                CP(S, "dve", STf[prt, vo], PS[prt, bSS * 512: bSS * 512 + 128][:, vo], bank(bSS), ["STf"])
                            else:
                                STT(S, STf[prt, vo], STf[prt, vo], PCOL[prt, c:c + 1], PS[prt, bSS * 512: bSS * 512 + 128][:, vo], ALU.mult, ALU.add,
                                    ["STf", "PCOL"] + bank(bSS), ["STf"])
                        CP(S, "pool", STB[:], STf[:], ["STf"], ["STB"])
                if RSTOP == 4:
                    raise _Stop()
                if RSTOP == 5:
                    raise _Stop()
                YT = F2
                for q4 in range(4):
                    bk = nb()
                    for j in range(4):
                        c = q4 * 4 + j
                        TR(S, PS[:, bk * 512 + j * 128: bk * 512 + (j + 1) * 128], Yt[:, c, :], cf("ident"), [("Y", c), "cst"], bank(bk))
                    CP(S, "act", YT[:, q4 * 512:(q4 + 1) * 512], PS[:, bk * 512:(bk + 1) * 512], bank(bk), ["F2"])
                ps, kb = small_mm([cf("blk")], [YT], ["cst", "F2"])
                STT(S, YT[:], ps, -1.0 / 64.0, YT[:], ALU.mult, ALU.add, kb + ["F2"], ["F2"])
                TT(S, "pool", F3[:], YT[:], YT[:], ALU.mult, ["F2"], ["F3"])
                ps, kb = small_mm([cf("blk")], [F3], ["cst", "F3"])
                ACT(S, F3[:], ps, AF.Sqrt, kb + ["F3"], ["F3"], scale=1.0 / 64.0, bias=EPSB[:, 1:2])
                S.op("dve", lambda e: e.reciprocal(out=F3[:], in_=F3[:]), ["F3"], ["F3"])
                TT(S, "dve", YT[:], YT[:], F3[:], ALU.mult, ["F2", "F3"], ["F2"])
                TS(S, "dve", YT[:], YT[:], col("lnx_w", hp), col("lnx_b", hp), ALU.mult, ALU.add, ["F2", "cols"], ["F2"])
                TT(S, "pool", YT[:], YT[:], FV[:], ALU.add, ["F2", "FV"], ["F2"])
                ps, kb = small_mm([GUP[:, 0, pc], GUP[0:32, 1, pc]], [SXG[:, 0, :], SXG[0:32, 1, :]], ["GUP", "SXG"])
                TT(S, "dve", OUTB[:], YT[:], ps, ALU.mult, ["F2"] + kb, ["RT"])
                S.dma("sp", MIXT[hp], OUTB[:], r=["RT"], w=[("MIXT", hp)])
                S.barrier()
              except _Stop:
                S.barrier()
        S.barrier()

    RSTDB = sb("rstdb", [128, SEQ], F32)
    HR = [sb("hr_%d" % i, [128, SEQ], F32) for i in range(2)]
    if "mixout" in phases:
        for c in range(16):
            S.dma("sp", XT[:, c, :], MIXT[c], w=[("XT", c)])
        for n in range(16):
            proj_fm(XTf, XTk, K16, w_mix[:, n * 128:(n + 1) * 128], 128, resid_epi(n))
        S.barrier()

    if "cross" in phases:
        with ExitStack() as pc:
            tc = lambda name, shape, dt: pc.enter_context(nc.sbuf_tensor(UN() + name, list(shape), dt))
            QC = tc("QC", [128, 4, SEQ], BF16)
            KC = tc("KC", [128, 4, NMEM], BF16)
            VC = tc("VC", [128, 2, 512], BF16)
            MT = tc("MT", [128, 16, NMEM], BF16)
            OCT = tc("OCT", [128, 4, SEQ], BF16)
            WKV = tc("WKV", [128, 16, 512], BF16)
            mtl = tc("mtl", [128, D], F32)
            msl = tc("msl", [128, D], F32)
            mjunk = tc("mjunk", [128, D], BF16)
            mss = tc("mss", [128, 2], F32)
            EE = [tc("E%d" % i, [128, 512], BF16) for i in range(2)]
            RD = tc("RD", [128, 512], F32)
            for mi in range(2):
                msl_ = slice(mi * 128, (mi + 1) * 128)
                S.dma("sp", mtl[:], mem[msl_, :], w=["mtl"])
                ACT(S, mjunk[:], mtl[:], AF.Square, ["mtl"], ["mjunk", ("mss", mi)], accum_out=mss[:, mi:mi + 1])
                ACT(S, mss[:, mi:mi + 1], mss[:, mi:mi + 1], AF.Sqrt, [("mss", mi)], [("mss", mi)], scale=1.0 / D, bias=EPSB[:, 0:1])
                S.op("dve", lambda e, mi=mi: e.reciprocal(out=mss[:, mi:mi + 1], in_=mss[:, mi:mi + 1]), [("mss", mi)], [("mss", mi)])
                ACT(S, msl[:], mtl[:], AF.Copy, ["mtl", ("mss", mi)], ["msl"], scale=mss[:, mi:mi + 1])
                for fg in range(4):
                    bk = fg
                    for j in range(4):
                        f = fg * 4 + j
                        TR(S, PS[:, bk * 512 + j * 128: bk * 512 + (j + 1) * 128], msl[:, f * 128:(f + 1) * 128], cf("ident"),
                           ["msl", "cst"], bank(bk))
                    for j in range(4):
                        f = fg * 4 + j
                        TS(S, "dve", MT[:, f, msl_], PS[:, bk * 512 + j * 128: bk * 512 + (j + 1) * 128], col("g_mem", f), None,
                           ALU.mult, None, bank(bk) + ["cols"], [("MT", f)])
            MTf = lambda c: MT[:, c, :]
            MTk = lambda c: [("MT", c)]
            for h in range(4):
                def kepi(ps, kb, h=h):
                    CP(S, "dve", KC[:, h, :], ps, kb, [("KC", h)])
                proj_fm(MTf, MTk, K16, w_kvc[:, h * 128:(h + 1) * 128], 128, kepi, ntok=NMEM)
            S.dma("pool", WKV[:], w_kvc[:, 512:1024].rearrange("(c p) m -> p c m", p=128), w=["WKV"])
            for mi in range(2):
                bk = 4 + mi
                for c in range(16):
                    MM(S, PS[:, bk * 512:(bk + 1) * 512], MT[:, c, mi * 128:(mi + 1) * 128], WKV[:, c, :], c == 0, c == 15,
                       [("MT", c), "WKV"], bank(bk))
                CP(S, "dve", VC[:, mi, :], PS[:, bk * 512:(bk + 1) * 512], bank(bk), [("VC", mi)])
            def xt_dest_c(n, src, keys):
                CP(S, "pool", XT[:, n, :], src, keys, [("XT", n)])
            rms_fm("g_cross", xt_dest_c)
            for h in range(4):
                def qepi(ps, kb, h=h):
                    CP(S, "act", QC[:, h, :], ps, kb, [("QC", h)])
                proj_fm(XTf, XTk, K16, w_qc[:, h * 128:(h + 1) * 128], 128, qepi)
            it = 0
            for h in range(4):
                for tg in range(4):
                    tsl = slice(tg * 512, (tg + 1) * 512)
                    for mi in range(2):
                        bs = 4 + (it % 2); it += 1
                        MM(S, PS[:, bs * 512:(bs + 1) * 512], KC[:, h, mi * 128:(mi + 1) * 128], QC[:, h, tsl], True, True,
                           [("KC", h), ("QC", h)], bank(bs))
                        ACT(S, EE[mi][:], PS[:, bs * 512:(bs + 1) * 512], AF.Exp, bank(bs), [("E", mi)], scale=128.0 ** -0.5)
                        MM(S, PS[:, 6 * 512:7 * 512], VC[:, mi, h * 128:(h + 1) * 128], EE[mi][:], mi == 0, mi == 1,
                           [("VC", mi), ("E", mi)], bank(6))
                        MM(S, PS[:, 7 * 512:8 * 512], onesb[:], EE[mi][:], mi == 0, mi == 1, ["onesb", ("E", mi)], bank(7))
                    S.op("dve", lambda e: e.reciprocal(out=RD[:], in_=PS[:, 7 * 512:8 * 512]), bank(7), ["RD"])
                    TT(S, "dve", OCT[:, h, tsl], PS[:, 6 * 512:7 * 512], RD[:], ALU.mult, bank(6) + ["RD"], [("OCT", h)])
            OCf = lambda c: OCT[:, c, :]
            OCk = lambda c: [("OCT", c)]
            for n in range(16):
                proj_fm(OCf, OCk, [128] * 4, w_oc[:, n * 128:(n + 1) * 128], 128, resid_epi(n))
        S.barrier()

    if "mlp" in phases:
        with ExitStack() as pm:
            AT = pm.enter_context(nc.sbuf_tensor(UN() + "AT", [128, 16, SEQ], BF16))
            RL = [pm.enter_context(nc.sbuf_tensor(UN() + "rl%d" % i, [128, SEQ], F32)) for i in range(2)]

            def xt_dest(n, src, keys):
                CP(S, "pool", XT[:, n, :], src, keys, [("XT", n)])
            rms_fm("g_mlp", xt_dest)
            ATf = lambda c: AT[:, c, :]
            ATk = lambda c: [("AT", c)]
            for gi in range(4):
                for ffc in range(16):
                    c0 = (gi * 16 + ffc) * 128

                    def epi(ps, kb, ffc=ffc):
                        b = ffc % 2
                        ACT(S, RL[b][:], ps, AF.Relu, kb, [("rl", b)])
                        TT(S, "pool", AT[:, ffc, :], RL[b][:], RL[b][:], ALU.mult, [("rl", b)], [("AT", ffc)])
                    proj_fm(XTf, XTk, K16, w_up[:, c0:c0 + 128], 128, epi)
                for n in range(16):
                    proj_fm(ATf, ATk, K16, w_dn[gi * 2048:(gi + 1) * 2048, n * 128:(n + 1) * 128], 128, resid_epi(n))
        S.barrier()

    with ExitStack() as pf:
        OT = [pf.enter_context(nc.sbuf_tensor(UN() + "ot%d" % i, [128, 16, 128], F32)) for i in range(2)]
        outv = out.rearrange("(tt p) f -> p tt f", p=128)

        def fin_dest(n, src, keys):
            b = n % 2
            for tg in range(4):
                bk = (n * 4 + tg) % 8
                for j in range(4):
                    tt = tg * 4 + j
                    TR(S, PS[:, bk * 512 + j * 128: bk * 512 + (j + 1) * 128], src[:, tt * 128:(tt + 1) * 128], cf("ident"),
                       keys + ["cst"], bank(bk))
                dst = OT[b][:, tg * 4:(tg + 1) * 4, :].rearrange("p a b -> p (a b)")
                CP(S, "act" if tg % 2 else "dve", dst, PS[:, bk * 512:(bk + 1) * 512], bank(bk), [("ot", b)])
            S.dma("sp", outv[:, :, n * 128:(n + 1) * 128], OT[b][:], r=[("ot", b)], w=[("out", n)])
        if VAR in ('V1', 'V2', 'V3'):
            S.dma("sp", out[0:128, :], HR[0][:], w=["o"])
        else:
            rms_fm("g_final", fin_dest)
    S.barrier()
    return nc


PHASES = ("dsa", "rwkv", "mixout", "cross", "mlp")


def make_in_maps(inp, nb=8):
    cols = _pack_cols(inp)
    consts = _consts()
    g = lambda k: np.ascontiguousarray(inp[k][0])
    shared = {"w_in": g("w_in"), "w_decay_up": g("w_decay_up"), "a_up": g("a_up"), "g_up": g("g_up"),
              "w_mix_out": g("w_mix_out"), "w_q_cross": g("w_q_cross"), "w_kv_cross": g("w_kv_cross"),
              "w_o_cross": g("w_o_cross"), "w_up": g("w_up"), "w_down": g("w_down"), "cols": cols, "consts": consts}
    maps = []
    for b in range(nb):
        m = dict(shared)
        m["x"] = np.ascontiguousarray(inp["x"][b]); m["mem"] = np.ascontiguousarray(inp["mem"][b])
        m["pos"] = np.ascontiguousarray(np.broadcast_to(inp["positions"][b:b + 1], (128, SEQ))).astype(np.int32)
        maps.append(m)
    return maps


def kernel(**inp):
    inp = {k: np.asarray(v) for k, v in inp.items()}
    nc = build()
    maps = make_in_maps(inp, 8)
    res = run_bass_kernel_spmd(nc, maps, core_ids=list(range(8)))
    return np.stack([np.asarray(r["out"]) for r in res.results]).astype(np.float32)
```
